# Optimizing a Trainium2 kernel written in Bass

```python
import math
import jax, jax.numpy as jnp
from jax import lax
import numpy as np

D_MODEL = 1024
BATCH = 8
SEQ = 2048
DEPTH = 1

N_HEADS_ATTN = 8
N_KV_HEADS = 2
HEAD_DIM_ATTN = 64
ATTN_GROUP = N_HEADS_ATTN // N_KV_HEADS
WINDOW = 128
ATTN_BLOCK = WINDOW
N_REL_BUCKETS = 32
REL_MAX_DISTANCE = 128
N_HEADS_MLSTM = 4
HEAD_DIM_MLSTM = 128
MLSTM_CHUNK = 64
CONV_WIDTH = 5
ATTN_Q_W = N_HEADS_ATTN * HEAD_DIM_ATTN
ATTN_KV_W = N_KV_HEADS * HEAD_DIM_ATTN
MLSTM_W = N_HEADS_MLSTM * HEAD_DIM_MLSTM
MLSTM_GATE_W = 2 * 2 * N_HEADS_MLSTM
MERGE_W = 2 * D_MODEL
IN_SPLITS = (ATTN_Q_W, ATTN_KV_W, ATTN_KV_W, MLSTM_W, MLSTM_W, MLSTM_W, MLSTM_W, MLSTM_GATE_W, MERGE_W)
D_IN_PROJ = sum(IN_SPLITS)
N_GROUPS = 4
EXPERTS_PER_GROUP = 8
N_EXPERTS = N_GROUPS * EXPERTS_PER_GROUP
TOP_K_EXPERT = 2
D_EXPERT = 512
MOE_BLOCK = 128
RMS_EPS = 1e-6
NEG_INF = -1e30

kernel_name = "hybrid_gqa_mlstm_hiermoe_encoder"


def rmsnorm(x, g):
    xf = x.astype(jnp.float32)
    y = xf * lax.rsqrt(jnp.mean(xf * xf, axis=-1, keepdims=True) + RMS_EPS)
    return (y * g.astype(jnp.float32)).astype(x.dtype)


def split_cols(t, sizes):
    outs, start = [], 0
    for s in sizes:
        outs.append(t[..., start:start + s])
        start += s
    return outs


def t5_bucket(rel):
    half = N_REL_BUCKETS // 2
    max_exact = half // 2
    sign = jnp.where(rel > 0, half, 0)
    n = jnp.abs(rel)
    nf = jnp.maximum(n, 1).astype(jnp.float32)
    large = max_exact + (jnp.log(nf / max_exact) / math.log(REL_MAX_DISTANCE / max_exact)
                         * (half - max_exact)).astype(jnp.int32)
    large = jnp.minimum(large, half - 1)
    return sign + jnp.where(n < max_exact, n, large)


def band_windows(t, nb):
    B = t.shape[0]
    tp = jnp.pad(t, ((0, 0), (ATTN_BLOCK, ATTN_BLOCK), (0, 0), (0, 0)))
    tp = tp.reshape(B, nb + 2, ATTN_BLOCK, t.shape[2], t.shape[3])
    return jnp.concatenate([tp[:, :-2], tp[:, 1:-1], tp[:, 2:]], axis=2)


def windowed_gqa(q, k, v, q_gain, k_gain, rel_table, sink):
    B, S = q.shape[0], q.shape[1]
    nb = S // ATTN_BLOCK
    q = q.reshape(B, S, N_HEADS_ATTN, HEAD_DIM_ATTN)
    k = k.reshape(B, S, N_KV_HEADS, HEAD_DIM_ATTN)
    v = v.reshape(B, S, N_KV_HEADS, HEAD_DIM_ATTN)
    q = rmsnorm(q, q_gain) * (HEAD_DIM_ATTN ** -0.5)
    k = rmsnorm(k, k_gain)
    qb = q.reshape(B, nb, ATTN_BLOCK, N_KV_HEADS, ATTN_GROUP, HEAD_DIM_ATTN)
    kw = band_windows(k, nb)
    vw = band_windows(v, nb)
    scores = jnp.einsum('bnqkgd,bnskd->bkgnqs', qb, kw).astype(jnp.float32)
    qi = jnp.arange(ATTN_BLOCK)
    ks = jnp.arange(3 * ATTN_BLOCK) - ATTN_BLOCK
    rel = ks[None, :] - qi[:, None]
    bias = jnp.transpose(rel_table[t5_bucket(rel)].astype(jnp.float32), (2, 0, 1))
    bias = bias.reshape(N_KV_HEADS, ATTN_GROUP, 1, ATTN_BLOCK, 3 * ATTN_BLOCK)
    kpos = jnp.arange(nb)[:, None, None] * ATTN_BLOCK + ks[None, None, :]
    valid = (jnp.abs(rel) <= WINDOW)[None] & (kpos >= 0) & (kpos < S)
    scores = jnp.where(valid, scores + bias, NEG_INF)
    sink_col = jnp.broadcast_to(sink.astype(jnp.float32).reshape(N_KV_HEADS, ATTN_GROUP, 1, 1, 1),
                                scores.shape[:-1] + (1,))
    probs = jax.nn.softmax(jnp.concatenate([scores, sink_col], axis=-1), axis=-1)[..., :-1]
    out = jnp.einsum('bkgnqs,bnskd->bnqkgd', probs.astype(v.dtype), vw)
    return out.reshape(B, S, ATTN_Q_W)


def mlstm_scan(q, k, v, ig, lf):
    B, H, S, d = q.shape
    L = MLSTM_CHUNK
    nc = S // L

    def chunks(t):
        return jnp.moveaxis(t.reshape((B, H, nc, L) + t.shape[3:]), 2, 0)

    tril = jnp.tril(jnp.ones((L, L), dtype=bool))

    def step(carry, inp):
        C, n, m = carry
        qc, kc, vc, igc, lfc = inp
        b = jnp.cumsum(lfc, axis=-1)
        dlog = jnp.where(tril, b[..., :, None] - b[..., None, :] + igc[..., None, :], -jnp.inf)
        g_inter = b + m[..., None]
        m_row = jnp.maximum(g_inter, jnp.max(dlog, axis=-1))
        w = jnp.exp(dlog - m_row[..., None])
        s = jnp.einsum('bhtd,bhsd->bhts', qc, kc) * w
        inter = jnp.exp(g_inter - m_row)
        num = jnp.einsum('bhts,bhsd->bhtd', s, vc) + inter[..., None] * jnp.einsum('bhtd,bhde->bhte', qc, C)
        den = jnp.sum(s, axis=-1) + inter * jnp.einsum('bhtd,bhd->bht', qc, n)
        h = num / jnp.maximum(jnp.abs(den), jnp.exp(-m_row))[..., None]
        b_last = b[..., -1]
        wlog = b_last[..., None] - b + igc
        m_new = jnp.maximum(b_last + m, jnp.max(wlog, axis=-1))
        decay = jnp.exp(b_last + m - m_new)
        ws = jnp.exp(wlog - m_new[..., None])
        C_new = decay[..., None, None] * C + jnp.einsum('bhs,bhsd,bhse->bhde', ws, kc, vc)
        n_new = decay[..., None] * n + jnp.einsum('bhs,bhsd->bhd', ws, kc)
        return (C_new, n_new, m_new), h

    init = (jnp.zeros((B, H, d, d), jnp.float32), jnp.zeros((B, H, d), jnp.float32),
            jnp.zeros((B, H), jnp.float32))
    _, h = lax.scan(step, init, (chunks(q), chunks(k), chunks(v), chunks(ig), chunks(lf)))
    return jnp.moveaxis(h, 0, 2).reshape(B, H, S, d)


def mlstm_branch(qm, km, vm, om, gates, conv_w, conv_b, gate_b, out_gain):
    B, S = qm.shape[0], qm.shape[1]
    qk = jnp.concatenate([qm, km], axis=-1)
    qk = lax.conv_general_dilated(qk, conv_w[:, None, :].astype(qk.dtype), (1,),
                                  [(CONV_WIDTH // 2, CONV_WIDTH // 2)],
                                  dimension_numbers=('NWC', 'WIO', 'NWC'),
                                  feature_group_count=qk.shape[-1]) + conv_b
    qk = jax.nn.silu(qk)
    qm, km = qk[..., :MLSTM_W], qk[..., MLSTM_W:]

    def heads(t):
        return t.reshape(B, S, N_HEADS_MLSTM, HEAD_DIM_MLSTM).transpose(0, 2, 1, 3).astype(jnp.float32)

    q = heads(qm)
    k = heads(km) * (HEAD_DIM_MLSTM ** -0.5)
    v = heads(vm)
    g = gates.astype(jnp.float32).reshape(B, S, 2, 2, N_HEADS_MLSTM) + gate_b.astype(jnp.float32)
    g = jnp.transpose(g, (2, 3, 0, 4, 1))
    h_f = mlstm_scan(q, k, v, g[0, 0], jax.nn.log_sigmoid(g[0, 1]))

    def flip(t):
        return jnp.flip(t, axis=2)

    h_b = flip(mlstm_scan(flip(q), flip(k), flip(v), flip(g[1, 0]), flip(jax.nn.log_sigmoid(g[1, 1]))))
    h = (h_f + h_b).transpose(0, 2, 1, 3)
    h = rmsnorm(h, out_gain)
    return h.reshape(B, S, MLSTM_W).astype(om.dtype) * jax.nn.sigmoid(om)


def hier_moe(h, w_rg, b_rg, w_re, b_re, w_gate, w_up, w_down):
    B, S, D = h.shape
    T = B * S
    xt = h.reshape(T, D)
    xf = xt.astype(jnp.float32)
    g_logits = xf @ w_rg.astype(jnp.float32) + b_rg.astype(jnp.float32)
    g_idx = jnp.argmax(g_logits, axis=-1).astype(jnp.int32)
    p_group = jnp.take_along_axis(jax.nn.softmax(g_logits, axis=-1), g_idx[:, None], axis=1)[:, 0]
    e_logits = (xf @ w_re.astype(jnp.float32) + b_re.astype(jnp.float32)).reshape(T, N_GROUPS, EXPERTS_PER_GROUP)
    e_in = jnp.take_along_axis(e_logits, g_idx[:, None, None], axis=1)[:, 0]
    top_v, top_i = lax.top_k(e_in, TOP_K_EXPERT)
    top_w = jax.nn.softmax(top_v, axis=-1) * p_group[:, None]
    expert_ids = (g_idx[:, None] * EXPERTS_PER_GROUP + top_i).reshape(-1).astype(jnp.int32)
    weights = top_w.reshape(-1)
    token_ids = jnp.repeat(jnp.arange(T, dtype=jnp.int32), TOP_K_EXPERT)
    A = T * TOP_K_EXPERT
    order = jnp.argsort(expert_ids)
    se, stok, sw = expert_ids[order], token_ids[order], weights[order]
    counts = jnp.bincount(expert_ids, length=N_EXPERTS)
    starts = jnp.cumsum(counts) - counts
    pcounts = ((counts + MOE_BLOCK - 1) // MOE_BLOCK) * MOE_BLOCK
    pends = jnp.cumsum(pcounts)
    pstarts = pends - pcounts
    dest = pstarts[se] + (jnp.arange(A, dtype=jnp.int32) - starts[se])
    P = A + N_EXPERTS * MOE_BLOCK
    nblk = P // MOE_BLOCK
    row_tok = jnp.zeros((P,), jnp.int32).at[dest].set(stok)
    row_w = jnp.zeros((P,), jnp.float32).at[dest].set(sw)
    blk_e = jnp.minimum(jnp.searchsorted(pends, jnp.arange(nblk) * MOE_BLOCK, side='right'),
                        N_EXPERTS - 1).astype(jnp.int32)
    xs = xt[row_tok].reshape(nblk, MOE_BLOCK, D)

    def expert_block(args):
        xb, e = args
        return (jax.nn.silu(xb @ w_gate[e]) * (xb @ w_up[e])) @ w_down[e]

    ys = lax.map(expert_block, (xs, blk_e)).reshape(P, D)
    y = jnp.zeros((T, D), ys.dtype).at[row_tok].add(ys * row_w[:, None].astype(ys.dtype))
    return y.reshape(B, S, D).astype(h.dtype)


def setup_inputs(seed: int = 0) -> dict:
    key = jax.random.key(seed)
    ks = jax.random.split(key, 24)

    def nrm(k, shape, scale):
        return jax.random.normal(k, shape, jnp.float32) * scale

    ib = nrm(ks[9], (DEPTH, 2, 1, N_HEADS_MLSTM), 0.1)
    fb = jnp.linspace(3.0, 6.0, N_HEADS_MLSTM, dtype=jnp.float32)[None, None, None, :] + nrm(ks[10], (DEPTH, 2, 1, N_HEADS_MLSTM), 0.01)
    return {
        'x': nrm(ks[0], (BATCH, SEQ, D_MODEL), 1.0),
        'norm1_g': 1.0 + nrm(ks[1], (DEPTH, D_MODEL), 0.01),
        'w_in': nrm(ks[2], (DEPTH, D_MODEL, D_IN_PROJ), D_MODEL ** -0.5),
        'attn_q_gain': 1.0 + nrm(ks[3], (DEPTH, HEAD_DIM_ATTN), 0.01),
        'attn_k_gain': 1.0 + nrm(ks[4], (DEPTH, HEAD_DIM_ATTN), 0.01),
        'attn_sink': nrm(ks[5], (DEPTH, N_HEADS_ATTN), 0.5),
        'rel_bias_table': nrm(ks[6], (N_REL_BUCKETS, N_HEADS_ATTN), 0.5),
        'mlstm_conv_w': nrm(ks[7], (DEPTH, CONV_WIDTH, 2 * MLSTM_W), CONV_WIDTH ** -0.5),
        'mlstm_conv_b': nrm(ks[8], (DEPTH, 2 * MLSTM_W), 0.01),
        'mlstm_gate_b': jnp.concatenate([ib, fb], axis=2),
        'mlstm_out_gain': 1.0 + nrm(ks[11], (DEPTH, N_HEADS_MLSTM, HEAD_DIM_MLSTM), 0.01),
        'w_branch_attn': nrm(ks[12], (DEPTH, ATTN_Q_W, D_MODEL), ATTN_Q_W ** -0.5),
        'w_branch_mlstm': nrm(ks[13], (DEPTH, MLSTM_W, D_MODEL), MLSTM_W ** -0.5),
        'merge_b': nrm(ks[14], (DEPTH, MERGE_W), 0.01),
        'w_out': nrm(ks[15], (DEPTH, D_MODEL, D_MODEL), D_MODEL ** -0.5),
        'norm2_g': 1.0 + nrm(ks[16], (DEPTH, D_MODEL), 0.01),
        'w_router_group': nrm(ks[17], (DEPTH, D_MODEL, N_GROUPS), D_MODEL ** -0.5),
        'b_router_group': nrm(ks[18], (DEPTH, N_GROUPS), 0.01),
        'w_router_expert': nrm(ks[19], (DEPTH, D_MODEL, N_EXPERTS), D_MODEL ** -0.5),
        'b_router_expert': nrm(ks[20], (DEPTH, N_EXPERTS), 0.01),
        'w_expert_gate': nrm(ks[21], (DEPTH, N_EXPERTS, D_MODEL, D_EXPERT), D_MODEL ** -0.5),
        'w_expert_up': nrm(ks[22], (DEPTH, N_EXPERTS, D_MODEL, D_EXPERT), D_MODEL ** -0.5),
        'w_expert_down': nrm(ks[23], (DEPTH, N_EXPERTS, D_EXPERT, D_MODEL), D_EXPERT ** -0.5),
    }


def reference(x, norm1_g, w_in, attn_q_gain, attn_k_gain, attn_sink, rel_bias_table,
              mlstm_conv_w, mlstm_conv_b, mlstm_gate_b, mlstm_out_gain,
              w_branch_attn, w_branch_mlstm, merge_b, w_out, norm2_g,
              w_router_group, b_router_group, w_router_expert, b_router_expert,
              w_expert_gate, w_expert_up, w_expert_down):
    for l in range(DEPTH):
        h = rmsnorm(x, norm1_g[l])
        proj = h @ w_in[l]
        qa, ka, va, qm, km, vm, om, gm, mg = split_cols(proj, IN_SPLITS)
        ya = windowed_gqa(qa, ka, va, attn_q_gain[l], attn_k_gain[l], rel_bias_table, attn_sink[l])
        ym = mlstm_branch(qm, km, vm, om, gm, mlstm_conv_w[l], mlstm_conv_b[l],
                          mlstm_gate_b[l], mlstm_out_gain[l])
        gates = jax.nn.sigmoid(mg + merge_b[l])
        u = gates[..., :D_MODEL] * (ya @ w_branch_attn[l]) + gates[..., D_MODEL:] * (ym @ w_branch_mlstm[l])
        x = x + u @ w_out[l]
        x = x + hier_moe(rmsnorm(x, norm2_g[l]), w_router_group[l], b_router_group[l],
                         w_router_expert[l], b_router_expert[l],
                         w_expert_gate[l], w_expert_up[l], w_expert_down[l])
    return x
```

```python
import math
import numpy as np
from contextlib import ExitStack
import concourse.bass as bass
import concourse.mybir as mybir
from concourse.bass_utils import run_bass_kernel_spmd

F32 = mybir.dt.float32
BF16 = mybir.dt.bfloat16
I32 = mybir.dt.int32
U8 = mybir.dt.uint8
AF = mybir.ActivationFunctionType
ALU = mybir.AluOpType
AX = mybir.AxisListType

S = 2048
D = 1024
NT = 16
DIN = 4880
NE = 32
CAPR = 256
NROWS = NE * CAPR + 128
TRASH = NE * CAPR
EPS = 1e-6
LNS = math.log(128.0 ** -0.5)
ENGS = ("pe", "act", "dve", "pool", "sp")

C_ID, C_J, C_MF, C_MB, C_TRI, C_ONE, C_IOTA, C_OHB, C_MROW, NCST = 0, 128, 256, 384, 512, 640, 768, 800, 1312, 1824


class Op:
    __slots__ = ("eng", "fn", "reads", "writes", "dma", "idx", "deps", "needs_inc", "incval", "dsem", "dval", "barrier", "bg")

    def __init__(self, eng, fn, reads, writes, dma):
        self.eng, self.fn, self.reads, self.writes, self.dma = eng, fn, tuple(reads), tuple(writes), dma
        self.deps = []
        self.needs_inc = False
        self.incval = 0
        self.dsem = None
        self.dval = 0
        self.barrier = False
        self.bg = False


class Prog:
    NDS = 56

    def __init__(self, nc):
        self.nc = nc
        self.ops = []

    frozen = False

    def op(self, eng, fn, reads=(), writes=(), dma=False):
        o = Op(eng, fn, reads, writes, dma)
        if not self.frozen:
            self.ops.append(o)
        return o

    def barrier(self):
        if self.frozen:
            return
        for e in ENGS:
            o = Op(e, None, (), (), False)
            o.barrier = True
            self.ops.append(o)

    def emit(self, stack):
        nc = self.nc
        engobj = {"pe": nc.tensor, "act": nc.scalar, "dve": nc.vector, "pool": nc.gpsimd, "sp": nc.sync}
        ops = self.ops
        lastw, readers = {}, {}
        last_on = {e: None for e in ENGS}
        all_dma = []
        for i, o in enumerate(ops):
            o.idx = i
            deps = set()
            rawset = set()
            if o.barrier:
                for e in ENGS:
                    if e != o.eng and last_on[e] is not None:
                        deps.add(last_on[e])
                deps.update(d_ for d_ in all_dma if not ops[d_].bg)
            else:
                for k in o.reads:
                    w = lastw.get(k)
                    if w is not None:
                        deps.add(w)
                        rawset.add(w)
                    if k.startswith("ps"):
                        deps.update(readers.get(k, ()))
                for k in o.writes:
                    w = lastw.get(k)
                    if w is not None:
                        deps.add(w)
                    deps.update(readers.get(k, ()))
            fdeps = []
            for j in deps:
                p = ops[j]
                if (not p.dma) and (not o.dma) and p.eng == o.eng:
                    if j not in rawset or o.eng == "pe":
                        continue
                fdeps.append(j)
            o.deps = fdeps
            if not o.barrier:
                for k in o.reads:
                    readers.setdefault(k, []).append(i)
                for k in o.writes:
                    lastw[k] = i
                    readers[k] = []
            if o.dma:
                all_dma.append(i)
            elif not o.barrier:
                last_on[o.eng] = i
            if o.barrier and o.eng == ENGS[-1]:
                all_dma = []
        for o in ops:
            for j in o.deps:
                if not ops[j].dma:
                    ops[j].needs_inc = True
        cnt = {e: 0 for e in ENGS}
        for o in ops:
            if o.dma or o.barrier:
                continue
            if o.needs_inc:
                cnt[o.eng] += 1
            o.incval = cnt[o.eng]
        esem = {e: stack.enter_context(nc.semaphore("s_" + e)) for e in ENGS}
        dsems = [stack.enter_context(nc.semaphore("d%d" % i)) for i in range(self.NDS)]
        duse = [0] * self.NDS
        nd = 0
        nd_sw = 0
        nd_hw = 0
        seen = {e: {f: 0 for f in ENGS} for e in ENGS}
        dseen = {e: {} for e in ENGS}
        nwait = 0
        for o in ops:
            e = engobj[o.eng]
            waits, dwaits = {}, {}
            for j in o.deps:
                p = ops[j]
                if p.dma:
                    if dseen[o.eng].get(p.dsem, 0) < p.dval:
                        dwaits[p.dsem] = max(dwaits.get(p.dsem, 0), p.dval)
                else:
                    v = p.incval
                    if seen[o.eng][p.eng] < v:
                        waits[p.eng] = max(waits.get(p.eng, 0), v)
            if o.dma:
                half = self.NDS // 2
                if o.eng == "pool":
                    s = nd_sw % half
                    nd_sw += 1
                else:
                    s = half + (nd_hw % half)
                    nd_hw += 1
                nd += 1
                if duse[s] > 0:
                    pv = duse[s] * 16
                    if dseen[o.eng].get(s, 0) < pv:
                        dwaits[s] = max(dwaits.get(s, 0), pv)
                duse[s] += 1
                o.dsem = s
                o.dval = duse[s] * 16
            for f, v in waits.items():
                e.wait_ge(esem[f], v)
                seen[o.eng][f] = v
                nwait += 1
            for s, v in dwaits.items():
                e.wait_ge(dsems[s], v)
                dseen[o.eng][s] = v
                nwait += 1
            if o.barrier:
                continue
            ins = o.fn(e)
            if o.dma:
                ins.then_inc(dsems[o.dsem], 16)
            elif o.needs_inc:
                ins.then_inc(esem[o.eng], 1)
        self.stats = dict(n_ops=len(ops), n_wait=nwait, incs=dict(cnt), n_dma=nd)
        return self.stats


class Arena:
    def __init__(self, nc, stack, nbytes):
        self.t = stack.enter_context(nc.sbuf_tensor("arena", [128, nbytes], U8))
        self.free = [(0, nbytes)]
        self.live = {}

    def alloc(self, name, shape, dt):
        esz = {F32: 4, BF16: 2, I32: 4}[dt]
        n = int(np.prod(shape)) * esz
        n_al = (n + 63) // 64 * 64
        for idx, (off, sz) in enumerate(self.free):
            if sz >= n_al:
                self.free[idx] = (off + n_al, sz - n_al)
                if self.free[idx][1] == 0:
                    del self.free[idx]
                break
        else:
            raise RuntimeError("arena OOM for %s (%d bytes); free=%s" % (name, n_al, self.free))
        assert name not in self.live, name
        self.live[name] = (off, n_al)
        v = self.t[:, off:off + n].bitcast(dt)
        if len(shape) == 2:
            v = v.rearrange("p (a b) -> p a b", a=shape[0])
        elif len(shape) == 3:
            v = v.rearrange("p (a b c) -> p a b c", a=shape[0], b=shape[1])
        elif len(shape) == 4:
            v = v.rearrange("p (a b c d) -> p a b c d", a=shape[0], b=shape[1], c=shape[2])
        return v

    def release(self, *names):
        for name in names:
            off, sz = self.live.pop(name)
            self.free.append((off, sz))
        self.free.sort()
        m = []
        for off, sz in self.free:
            if m and m[-1][0] + m[-1][1] == off:
                m[-1] = (m[-1][0], m[-1][1] + sz)
            else:
                m.append((off, sz))
        self.free = m


def t5_bucket_np(rel):
    half, max_exact = 16, 8
    sign = np.where(rel > 0, half, 0)
    n = np.abs(rel)
    nf = np.maximum(n, 1).astype(np.float32)
    large = max_exact + (np.log(nf / max_exact) / math.log(128 / max_exact) * (half - max_exact)).astype(np.int32)
    large = np.minimum(large, half - 1)
    return sign + np.where(n < max_exact, n, large)


def make_consts():
    c = np.zeros((128, NCST), np.float32)
    p = np.arange(128)[:, None]
    q = np.arange(128)[None, :]
    c[:, C_ID:C_ID + 128] = (p == q)
    c[:, C_J:C_J + 128] = (p + q == 127)
    c[:, C_MF:C_MF + 128] = (p <= q)
    c[:, C_MB:C_MB + 128] = (p >= q)
    c[:, C_TRI:C_TRI + 128] = (p < q)
    c[:, C_ONE:C_ONE + 128] = 1.0
    c[:, C_IOTA:C_IOTA + 32] = np.arange(32)[None, :] * float(CAPR)
    r = np.arange(512)
    rel = r - 255
    valid = np.abs(rel) <= 128
    bk = t5_bucket_np(rel)
    for b in range(32):
        c[b, C_OHB:C_OHB + 512] = ((bk == b) & valid).astype(np.float32)
    c[0:8, C_MROW:C_MROW + 512] = np.where(valid, 0.0, -30000.0)[None, :]
    return c


def build_program(debug=(), stop=None):
    nc = bass.Bass("TRN2", target_bir_lowering=False)

    def din(name, shape, dt=F32):
        return nc.dram_tensor(name, list(shape), dt, kind="ExternalInput").ap()

    def dscr(name, shape, dt):
        return nc.dram_tensor(name, list(shape), dt, kind="Internal").ap()

    x_d = din("x", [S, D])
    cst_d = din("cst", [128, NCST])
    g1_d = din("norm1_g", [D])
    win_d = din("w_in", [D, DIN])
    gq_d = din("attn_q_gain", [64])
    gk_d = din("attn_k_gain", [64])
    sink_d = din("attn_sink", [8])
    tab_d = din("rel_bias_table", [32, 8])
    cw_d = din("mlstm_conv_w", [5, 1024])
    cb_d = din("mlstm_conv_b", [1024])
    gb_d = din("mlstm_gate_b", [16])
    og_d = din("mlstm_out_gain", [512])
    wa_d = din("w_branch_attn", [512, D])
    wm_d = din("w_branch_mlstm", [512, D])
    mb_d = din("merge_b", [2048])
    wo_d = din("w_out", [D, D])
    g2_d = din("norm2_g", [D])
    wrg_d = din("w_router_group", [D, 4])
    brg_d = din("b_router_group", [4])
    wre_d = din("w_router_expert", [D, 32])
    bre_d = din("b_router_expert", [32])
    weg_d = din("w_expert_gate", [NE, D, 512])
    weu_d = din("w_expert_up", [NE, D, 512])
    wed_d = din("w_expert_down", [NE, 512, D])
    out_d = nc.dram_tensor("out", [S, D], F32, kind="ExternalOutput").ap()

    hT_d = dscr("hT_scr", [128, 8 * S], BF16)
    sig_d = dscr("sig_scr", [S, 512], F32)
    yaT_d = dscr("yaT_scr", [128, 4 * S], BF16)
    x1_d = dscr("x1_scr", [S, D], F32)
    xs_d = dscr("xs_scr", [NROWS, D], BF16)
    ys_d = dscr("ys_scr", [NROWS, D], F32)
    dec_d = dscr("dec_scr", [36 * 16], F32)
    wgs_d = dscr("wgs_scr", [NE, 128, 4096], BF16)
    wus_d = dscr("wus_scr", [NE, 128, 4096], BF16)
    wds_d = dscr("wds_scr", [NE, 128, 4096], BF16)
    ev_d = dscr("ev_scr", [8, 512], F32)

    dbg_out = {}

    def dbg(name, shape, dt=F32):
        dbg_out[name] = nc.dram_tensor("dbg_" + name, list(shape), dt, kind="ExternalOutput").ap()
        return dbg_out[name]

    P = Prog(nc)
    outkeys = []

    def checkpoint(name):
        if stop == name:
            P.frozen = True
    with ExitStack() as st:
        A = Arena(nc, st, 204 * 1024)
        PS = [st.enter_context(nc.psum_tensor("ps%d" % b, [128, 512], F32)) for b in range(8)]

        def psf(b):
            return PS[b][:]

        def psb(b):
            return PS[b][:].bitcast(BF16)

        def dma(q, out, in_, reads, writes):
            P.op(q, lambda e: e.dma_start(out=out, in_=in_), reads=reads, writes=writes, dma=True)

        def mmgroup(items, reads, writes):
            def fn(e):
                ins = None
                for (o_, l_, r_, s0, s1) in items:
                    ins = e.matmul(o_, l_, r_, start=s0, stop=s1)
                return ins
            P.op("pe", fn, reads=reads, writes=writes)

        def trgroup(items, reads, writes):
            def fn(e):
                ins = None
                for (o_, i_, id_) in items:
                    ins = e.transpose(o_, i_, id_)
                return ins
            P.op("pe", fn, reads=reads, writes=writes)

        def act(out, in_, func, reads, writes, bias=0.0, scale=1.0, accum_out=None):
            if accum_out is None:
                P.op("act", lambda e: e.activation(out=out, in_=in_, func=func, bias=bias, scale=scale), reads=reads, writes=writes)
            else:
                P.op("act", lambda e: e.activation(out=out, in_=in_, func=func, bias=bias, scale=scale, accum_out=accum_out), reads=reads, writes=writes)

        def tt(eng, out, in0, in1, op, reads, writes):
            P.op(eng, lambda e: e.tensor_tensor(out, in0, in1, op), reads=reads, writes=writes)

        def ts(eng, out, in0, s1, s2, op0, op1, reads, writes):
            if op1 is None:
                P.op(eng, lambda e: e.tensor_scalar(out, in0, s1, None, op0), reads=reads, writes=writes)
            else:
                P.op(eng, lambda e: e.tensor_scalar(out, in0, s1, s2, op0, op1), reads=reads, writes=writes)

        def stt(out, in0, scalar, in1, op0, op1, reads, writes, accum_out=None):
            if accum_out is None:
                P.op("dve", lambda e: e.scalar_tensor_tensor(out, in0, scalar, in1, op0, op1), reads=reads, writes=writes)
            else:
                P.op("dve", lambda e: e.scalar_tensor_tensor(out, in0, scalar, in1, op0, op1, accum_out=accum_out), reads=reads, writes=writes)

        def cp(eng, out, in_, reads, writes):
            if eng == "act":
                P.op("act", lambda e: e.activation(out=out, in_=in_, func=AF.Copy), reads=reads, writes=writes)
            else:
                P.op(eng, lambda e: e.tensor_copy(out, in_), reads=reads, writes=writes)

        def recip(out, in_, reads, writes):
            P.op("dve", lambda e: e.reciprocal(out, in_), reads=reads, writes=writes)

        def memset(eng, ap, val, writes):
            P.op(eng, lambda e: e.memset(ap, val), writes=writes)

        def wview(w_d):
            return w_d.rearrange("(c p) n -> p c n", p=128)

        identf = A.alloc("identf", [128], F32)
        Jf = A.alloc("Jf", [128], F32)
        maskf = A.alloc("maskf", [128], F32)
        maskb = A.alloc("maskb", [128], F32)
        identb = A.alloc("identb", [128], BF16)
        trib = A.alloc("trib", [128], BF16)
        onesb = A.alloc("onesb", [128], BF16)
        basee = A.alloc("basee", [32], F32)
        g1b = A.alloc("g1b", [D], F32)
        g2b = A.alloc("g2b", [D], F32)
        prm = A.alloc("prm", [64], F32)
        gbc = A.alloc("gbc", [2], F32)
        dma("sp", identf, cst_d[:, C_ID:C_ID + 128], [], ["identf"])
        dma("sp", Jf, cst_d[:, C_J:C_J + 128], [], ["Jf"])
        dma("sp", maskf, cst_d[:, C_MF:C_MF + 128], [], ["maskf"])
        dma("sp", maskb, cst_d[:, C_MB:C_MB + 128], [], ["maskb"])
        dma("sp", basee, cst_d[:, C_IOTA:C_IOTA + 32], [], ["basee"])
        dma("pool", identb, cst_d[:, C_ID:C_ID + 128], [], ["identb"])
        dma("pool", trib, cst_d[:, C_TRI:C_TRI + 128], [], ["trib"])
        dma("pool", onesb, cst_d[:, C_ONE:C_ONE + 128], [], ["onesb"])
        dma("sp", g1b, g1_d.partition_broadcast(128), [], ["g1b"])
        dma("sp", g2b, g2_d.partition_broadcast(128), [], ["g2b"])
        memset("dve", gbc, 0.0, ["gbc"])
        for d_ in range(2):
            for f_ in range(2):
                dma("sp", gbc[32 * d_:32 * d_ + 4, f_:f_ + 1], bass.AP(gb_d.tensor, d_ * 8 + f_ * 4, [[1, 4], [1, 1]]), ["gbc"], ["gbc"])
        zt = A.alloc("zt", [D], F32)
        memset("dve", zt, 0.0, ["zt"])
        ztb = zt.bitcast(BF16)[:, 0:D]
        pst = A.alloc("pst", [128], F32)
        dma("sp", pst[0:40, :], cw_d.rearrange("j (g p) -> (j g) p", p=128), [], ["pst_a"])
        dma("sp", pst[40:48, :], cb_d.rearrange("(g p) -> g p", p=128), [], ["pst_b"])
        dma("sp", pst[48:64, :], mb_d.rearrange("(g p) -> g p", p=128), [], ["pst_c"])
        trgroup([(psf(0)[:, 0:64], pst[0:64, :], identf[0:64, 0:64])], ["pst_a", "pst_b", "pst_c", "identf"], ["ps0"])
        cp("dve", prm, psf(0)[:, 0:64], ["ps0"], ["prm"])

        hT = A.alloc("hT", [8, S], BF16)
        xts = [A.alloc("xt%d" % i, [D], F32) for i in range(2)]
        hbfs = [A.alloc("hbf%d" % i, [D], BF16) for i in range(2)]
        junk = A.alloc("junk", [D], BF16)
        ss1 = A.alloc("ss1", [NT], F32)
        rs1 = A.alloc("rs1", [NT], F32)
        for i in range(NT):
            xt, hb = xts[i % 2], hbfs[i % 2]
            kx, kh = "xt%d" % (i % 2), "hbf%d" % (i % 2)
            dma("sp", xt, x_d[i * 128:(i + 1) * 128, :], [], [kx])
            act(junk, xt, AF.Square, [kx], ["junk", "ss1.%d" % i], accum_out=ss1[:, i:i + 1])
            act(rs1[:, i:i + 1], ss1[:, i:i + 1], AF.Sqrt, ["ss1.%d" % i], ["rs1.%d" % i], bias=EPS, scale=1.0 / D)
            recip(rs1[:, i:i + 1], rs1[:, i:i + 1], ["rs1.%d" % i], ["rs1.%d" % i])
            stt(hb, xt, rs1[:, i:i + 1], g1b, ALU.mult, ALU.mult, [kx, "rs1.%d" % i, "g1b"], [kh])
            b = i % 2
            trgroup([(psb(b)[:, c * 128:(c + 1) * 128], hb[:, c * 128:(c + 1) * 128], identb) for c in range(8)],
                    [kh, "identb"], ["ps%d" % b])
            cp("act" if i % 2 else "dve", hT[:, :, i * 128:(i + 1) * 128],
               psb(b).rearrange("p (c t) -> p c t", c=8), ["ps%d" % b], ["hT.%d" % i])
        P.barrier()
        A.release("xt0", "xt1", "hbf0", "hbf1", "junk", "ss1", "rs1", "pst")
        if "hT" in debug:
            d_ = dbg("hT", [128, 8 * S], BF16)
            dma("sp", d_, hT.rearrange("p c t -> p (c t)"), ["hT.%d" % i for i in range(NT)], ["dbg_hT"])
            outkeys.append("dbg_hT")
        hT_keys = ["hT.%d" % i for i in range(NT)]

        def hT_tc_keys(tc):
            return ["hT.%d" % i for i in range(tc * 4, tc * 4 + 4)]

        dma("sp", xs_d.rearrange("(r p) d -> p r d", p=128),
            ztb.unsqueeze(1).broadcast_to([128, NROWS // 128, D]), ["zt"], ["xs"])
        dma("sp", ys_d[TRASH:TRASH + 128, :], zt, ["zt"], ["ys_trash"])
        wbuf = [A.alloc("wbuf%d" % i, [8, 512], BF16) for i in range(2)]
        wcnt = [0]

        def load_w(c0, ncols):
            i = wcnt[0] % 2
            wcnt[0] += 1
            dma("pool", wbuf[i][:, :, 0:ncols], wview(win_d)[:, :, c0:c0 + ncols], [], ["wbuf%d" % i])
            return wbuf[i], "wbuf%d" % i

        qT = A.alloc("qT", [4, S], BF16)
        kT = A.alloc("kT", [S], BF16)
        vaug = A.alloc("vaug", [NT, 2, 65], BF16)
        qmT = A.alloc("qmT", [4, S], BF16)
        kmT = A.alloc("kmT", [4, S], BF16)
        vaugm = A.alloc("vaugm", [NT, 4, 129], BF16)
        GW01 = [A.alloc("gw%d" % i, [S], F32) for i in range(2)]
        gqb = A.alloc("gqb", [64], F32)
        gkb = A.alloc("gkb", [64], F32)
        sq = A.alloc("sq", [512], F32)
        tmpq = A.alloc("tmpq", [512], F32)
        qns = [A.alloc("qn%d" % i, [512], BF16) for i in range(2)]
        ssq = A.alloc("ssq", [8], F32)
        sgo = [A.alloc("sgo%d" % i, [512], F32) for i in range(2)]
        dma("sp", gqb, gq_d.partition_broadcast(128), [], ["gqb"])
        dma("sp", gkb, gk_d.partition_broadcast(128), [], ["gkb"])
        act(gqb, gqb, AF.Copy, ["gqb"], ["gqb"], scale=0.125)
        memset("dve", vaug[:, :, :, 64:65], 1.0, ["vaug1"])
        memset("dve", vaugm[:, :, :, 128:129], 1.0, ["vaugm1"])

        def tokmajor(c0, ncols, bank0, consume, consume_b=None):
            w, wk = load_w(c0, ncols)
            for i in range(NT):
                b = i % 4
                mmgroup([(psf(b)[:, 0:ncols], hT[:, c, i * 128:(i + 1) * 128], w[:, c, 0:ncols], c == 0, c == 7) for c in range(8)],
                        ["hT.%d" % i, wk], ["ps%d" % b])
                consume(i, b)
                if consume_b is not None and i >= 1:
                    consume_b(i - 1, (i - 1) % 4)
            if consume_b is not None:
                consume_b(NT - 1, (NT - 1) % 4)

        def qk_norm(src, nh, gain, gk_, out_bf, reads, tag):
            act(sq[:, 0:nh * 64], src, AF.Square, reads, ["sq"])
            P.op("dve", lambda e: e.tensor_reduce(ssq[:, 0:nh], sq[:, 0:nh * 64].rearrange("p (h d) -> p h d", h=nh), AX.X, ALU.add),
                 reads=["sq"], writes=["ssq"])
            act(ssq[:, 0:nh], ssq[:, 0:nh], AF.Sqrt, ["ssq"], ["ssq"], bias=EPS, scale=1.0 / 64)
            recip(ssq[:, 0:nh], ssq[:, 0:nh], ["ssq"], ["ssq"])
            tt("dve", tmpq[:, 0:nh * 64].rearrange("p (h d) -> p h d", h=nh), src.rearrange("p (h d) -> p h d", h=nh),
               ssq[:, 0:nh].unsqueeze(2).broadcast_to([128, nh, 64]), ALU.mult, reads + ["ssq"], ["tmpq"])
            if nh == 8:
                tt("dve", out_bf.rearrange("p (a g d) -> p g a d", a=4, g=2), tmpq.rearrange("p (g a d) -> p g a d", g=2, a=4),
                   gain.unsqueeze(1).unsqueeze(1).broadcast_to([128, 2, 4, 64]), ALU.mult, ["tmpq", gk_], [tag])
            else:
                tt("dve", out_bf.rearrange("p (h d) -> p h d", h=nh), tmpq[:, 0:nh * 64].rearrange("p (h d) -> p h d", h=nh),
                   gain.unsqueeze(1).broadcast_to([128, nh, 64]), ALU.mult, ["tmpq", gk_], [tag])

        def consume_q(i, b):
            qk_norm(psf(b), 8, gqb, "gqb", qns[i % 2], ["ps%d" % b], "qn%d" % (i % 2))

        def consume_q_b(i, b):
            tb = 4 + (i % 2)
            q_, qk_ = qns[i % 2], "qn%d" % (i % 2)
            trgroup([(psb(tb)[:, a * 128:(a + 1) * 128], q_[:, a * 128:(a + 1) * 128], identb) for a in range(4)], [qk_, "identb"], ["ps%d" % tb])
            cp("act", qT[:, :, i * 128:(i + 1) * 128], psb(tb)[:, 0:512].rearrange("p (a t) -> p a t", a=4), ["ps%d" % tb], ["qT.%d" % i])

        def consume_kv(i, b):
            qk_norm(psf(b)[:, 0:128], 2, gkb, "gkb", qns[i % 2][:, 0:128], ["ps%d" % b], "qn%d" % (i % 2))
            cp("act", vaug[:, i, :, 0:64], psf(b)[:, 128:256].rearrange("p (g d) -> p g d", g=2), ["ps%d" % b], ["vaug.%d" % i])

        def consume_kv_b(i, b):
            tb = 4 + (i % 2)
            q_, qk_ = qns[i % 2], "qn%d" % (i % 2)
            trgroup([(psb(tb)[:, 0:128], q_[:, 0:128], identb)], [qk_, "identb"], ["ps%d" % tb])
            cp("act", kT[:, i * 128:(i + 1) * 128], psb(tb)[:, 0:128], ["ps%d" % tb], ["kT.%d" % i])

        def consume_vm(i, b):
            cp("act" if i % 2 else "dve", vaugm[:, i, :, 0:128], psf(b).rearrange("p (h d) -> p h d", h=4), ["ps%d" % b], ["vaugm.%d" % i])

        def consume_om(i, b):
            sg_, sk = sgo[i % 2], "sgo%d" % (i % 2)
            act(sg_, psf(b), AF.Sigmoid, ["ps%d" % b], [sk])
            dma("sp", sig_d[i * 128:(i + 1) * 128, :], sg_, [sk], ["sig_d.%d" % i])

        tokmajor(0, 512, 0, consume_q, consume_q_b)
        tokmajor(512, 256, 2, consume_kv, consume_kv_b)
        tokmajor(1792, 512, 0, consume_vm)
        tokmajor(2304, 512, 2, consume_om)

        xpb = [A.alloc("xpb%d" % i, [S + 4], BF16) for i in range(2)]
        dg = A.alloc("dg", [8, 5, 128], BF16)
        for g in range(8):
            for j in range(5):
                ts("pool", dg[:, g, j, :], identb, prm[:, j * 8 + g:j * 8 + g + 1], None, ALU.mult, None, ["identb", "prm"], ["dg.%d" % g])
        for i_ in range(2):
            memset("dve", xpb[i_][:, 0:2], 0.0, ["xpb%d_l" % i_])
            memset("dve", xpb[i_][:, S + 2:S + 4], 0.0, ["xpb%d_r" % i_])
        wqk = {}

        def cv_proj(g):
            qk, h = g // 4, g % 4
            if h == 0:
                wqk[qk] = load_w(768 + 512 * qk, 512)
            w, wk = wqk[qk]
            xb_ = xpb[g % 2]
            for tc in range(4):
                b = 6 + (tc % 2)
                mmgroup([(psf(b), w[:, c, h * 128:(h + 1) * 128], hT[:, c, tc * 512:(tc + 1) * 512], c == 0, c == 7) for c in range(8)],
                        hT_tc_keys(tc) + [wk], ["ps%d" % b])
                cp("act", xb_[:, 2 + tc * 512:2 + (tc + 1) * 512], psf(b), ["ps%d" % b], ["xpb%d.%d" % (g % 2, tc)])

        def cv_conv(g):
            qk, h = g // 4, g % 4
            dst = qmT if qk == 0 else kmT
            xb_ = xpb[g % 2]
            xk = ["xpb%d.%d" % (g % 2, t) for t in range(4)] + ["xpb%d_l" % (g % 2), "xpb%d_r" % (g % 2)]
            for tc in range(4):
                b = 4 + (tc % 2)
                mmgroup([(psf(b), dg[:, g, j, :], xb_[:, tc * 512 + j:tc * 512 + j + 512], j == 0, j == 4) for j in range(5)],
                        xk + ["dg.%d" % g], ["ps%d" % b])
                act(dst[:, h, tc * 512:(tc + 1) * 512], psf(b), AF.Silu, ["ps%d" % b, "prm"],
                    ["%s.%d" % ("qmT" if qk == 0 else "kmT", h)], bias=prm[:, 40 + g:41 + g])

        for g in range(9):
            if g < 8:
                cv_proj(g)
            if g >= 1:
                cv_conv(g - 1)
        w, wk = load_w(2816, 16)
        wpad = A.alloc("wpad", [8, 2, 36], BF16)
        memset("dve", wpad, 0.0, ["wpad"])
        for d_ in range(2):
            for f_ in range(2):
                cp("dve", wpad[:, :, f_, 32 * d_:32 * d_ + 4], w[:, :, d_ * 8 + f_ * 4:d_ * 8 + f_ * 4 + 4], [wk, "wpad"], ["wpad"])
        for f_ in range(2):
            memset("dve", GW01[f_], 0.0, ["gw%d.0" % f_, "gw%d.1" % f_])
        for tc in range(4):
            for f_ in range(2):
                b = 6 + f_
                mmgroup([(psf(b)[0:36, :], wpad[:, c, f_, :], hT[:, c, tc * 512:(tc + 1) * 512], c == 0, c == 7) for c in range(8)],
                        hT_tc_keys(tc) + ["wpad"], ["ps%d" % b])
                act(GW01[f_][0:36, tc * 512:(tc + 1) * 512], psf(b)[0:36, :], AF.Identity, ["ps%d" % b, "gbc"],
                    ["gw%d.0" % f_, "gw%d.1" % f_], bias=gbc[0:36, f_:f_ + 1])
        dma("sp", hT_d, hT.rearrange("p c t -> p (c t)"), hT_keys, ["hT_d"])
        P.barrier()
        A.release("hT", "wbuf0", "wbuf1", "sq", "tmpq", "qn0", "qn1", "ssq", "sgo0", "sgo1", "xpb0", "xpb1", "dg", "gqb", "gkb", "wpad")

        if "p2" in debug:
            for nm, ap_, shp, dt_, keys in [
                ("qT", qT.rearrange("p a t -> p (a t)"), [128, 4 * S], BF16, []),
                ("kT", kT, [128, S], BF16, []),
                ("vaug", vaug.rearrange("p a b c -> p (a b c)"), [128, NT * 2 * 65], BF16, []),
                ("qmT", qmT.rearrange("p a t -> p (a t)"), [128, 4 * S], BF16, []),
                ("kmT", kmT.rearrange("p a t -> p (a t)"), [128, 4 * S], BF16, []),
                ("vaugm", vaugm.rearrange("p a b c -> p (a b c)"), [128, NT * 4 * 129], BF16, []),
            ]:
                d_ = dbg(nm, shp, dt_)
                dma("sp", d_, ap_, [], ["dbg_" + nm])
                outkeys.append("dbg_" + nm)
            d_ = dbg("sig", [S, 512], F32)
            dma("sp", d_, sig_d, ["sig_d.%d" % i for i in range(NT)], ["dbg_sig"])
            outkeys.append("dbg_sig")


        tabs = A.alloc("tabs", [8], F32)
        ohb = A.alloc("ohb", [512], F32)
        mrow = A.alloc("mrow", [512], F32)
        evec = A.alloc("evec", [512], F32)
        esink = A.alloc("esink", [8], F32)
        hk = A.alloc("hk", [8, 3, 128], F32)
        expb = A.alloc("expb", [3, 8, 128], F32)
        dma("sp", tabs[0:32, :], tab_d, [], ["tabs"])
        dma("sp", ohb[0:32, :], cst_d[0:32, C_OHB:C_OHB + 512], [], ["ohb"])
        dma("sp", mrow[0:8, :], cst_d[0:8, C_MROW:C_MROW + 512], [], ["mrow"])
        dma("sp", esink, sink_d.partition_broadcast(128), [], ["esink"])
        act(esink, esink, AF.Exp, ["esink"], ["esink"])
        mmgroup([(psf(0)[0:8, :], tabs[0:32, :], ohb[0:32, :], True, True)], ["tabs", "ohb"], ["ps0"])
        tt("dve", evec[0:8, :], psf(0)[0:8, :], mrow[0:8, :], ALU.add, ["ps0", "mrow"], ["evec"])
        act(evec[0:8, :], evec[0:8, :], AF.Exp, ["evec"], ["evec"])
        dma("sp", ev_d, evec[0:8, :], ["evec"], ["ev_d"])
        for h in range(8):
            dma("sp", hk[:, h, :, :], bass.AP(ev_d.tensor, h * 512, [[1, 128], [128, 3], [1, 128]]), ["ev_d"], ["hk.%d" % h])
        for j in range(3):
            for hh in range(2):
                b = 1 + ((j * 2 + hh) % 2)
                mmgroup([(psf(b)[:, a * 128:(a + 1) * 128], hk[:, hh * 4 + a, j, :], Jf, True, True) for a in range(4)],
                        ["hk.%d" % (hh * 4 + a) for a in range(4)] + ["Jf"], ["ps%d" % b])
                cp("dve", expb[:, j, hh * 4:hh * 4 + 4, :], psf(b).rearrange("p (a q) -> p a q", a=4), ["ps%d" % b], ["expb"])
        pck = 0
        for e_ in range(NE):
            for (dst_, src_, c_, kn_) in ((wgs_d, weg_d, 8, "wgs"), (wus_d, weu_d, 8, "wus"), (wds_d, wed_d, 4, "wds")):
                o__ = P.op("pool", lambda e, o_=dst_[e_], i_=src_[e_].rearrange("(p c) n -> p (c n)", c=c_): e.dma_start(out=o_, in_=i_),
                           reads=["expb", "esink"] + (["pc.%d" % (pck - 2)] if pck >= 2 else []),
                           writes=["%s.%d" % (kn_, e_), "pc.%d" % pck], dma=True)
                o__.bg = True
                pck += 1
        yaT = A.alloc("yaT", [4, S], BF16)
        Es = [A.alloc("Es%d" % i, [512], F32) for i in range(2)]
        Pj = [A.alloc("Pj%d" % i, [512], BF16) for i in range(6)]
        yat = [A.alloc("yat%d" % i, [512], BF16) for i in range(2)]
        dens = A.alloc("dens", [8], F32)
        qkeys = ["qT.%d" % i for i in range(NT)]
        ecount = 0
        def at_sa(it):
            n, g = it // 2, it % 2
            js = [j for j in range(3) if 0 <= n - 1 + j < NT]
            for j in js:
                kb = n - 1 + j
                b = (it % 2) * 3 + j
                mmgroup([(psf(b), kT[64 * g:64 * g + 64, kb * 128:(kb + 1) * 128],
                          qT[64 * g:64 * g + 64, :, n * 128:(n + 1) * 128], True, True)],
                        ["kT.%d" % kb, "qT.%d" % n], ["ps%d" % b])
                ei = (it * 3 + j) % 2
                E_, ek = Es[ei], "Es%d" % ei
                act(E_, psf(b), AF.Exp, ["ps%d" % b], [ek])
                pj, pk = Pj[(it % 2) * 3 + j], "Pj%d" % ((it % 2) * 3 + j)
                tt("dve", pj.rearrange("p (a q) -> p a q", a=4), E_.rearrange("p (a q) -> p a q", a=4),
                   expb[:, j, 4 * g:4 * g + 4, :], ALU.mult, [ek, "expb"], [pk])

        def at_sb(it):
            n, g = it // 2, it % 2
            ya_t, yk = yat[n % 2], "yat%d" % (n % 2)
            js = [j for j in range(3) if 0 <= n - 1 + j < NT]
            ob = 6 + (it % 2)
            items = []
            for a in range(4):
                for jj, j in enumerate(js):
                    kb = n - 1 + j
                    items.append((psf(ob)[:, a * 65:(a + 1) * 65], Pj[(it % 2) * 3 + j][:, a * 128:(a + 1) * 128],
                                  vaug[:, kb, g, :], jj == 0, jj == len(js) - 1))
            mmgroup(items, ["Pj%d" % ((it % 2) * 3 + j) for j in js] + ["vaug.%d" % (n - 1 + j) for j in js] + ["vaug1"], ["ps%d" % ob])
            o3 = psf(ob)[:, 0:260].rearrange("p (a d) -> p a d", a=4)
            dk = "dens.%d" % g
            tt("dve", dens[:, 4 * g:4 * g + 4].unsqueeze(2), o3[:, :, 64:65], esink[:, 4 * g:4 * g + 4].unsqueeze(2), ALU.add,
               ["ps%d" % ob, "esink"], [dk])
            recip(dens[:, 4 * g:4 * g + 4], dens[:, 4 * g:4 * g + 4], [dk], [dk])
            tt("dve", ya_t[:, g * 256:(g + 1) * 256].rearrange("p (a d) -> p a d", a=4), o3[:, :, 0:64],
               dens[:, 4 * g:4 * g + 4].unsqueeze(2).broadcast_to([128, 4, 64]), ALU.mult, ["ps%d" % ob, dk], [yk + ".%d" % g])

        def at_sc(n):
            ya_t, yk = yat[n % 2], "yat%d" % (n % 2)
            tb = 6 + (n % 2)
            trgroup([(psb(tb)[:, c * 128:(c + 1) * 128], ya_t[:, c * 128:(c + 1) * 128], identb) for c in range(4)],
                    [yk + ".0", yk + ".1", "identb"], ["ps%d" % tb])
            cp("act", yaT[:, :, n * 128:(n + 1) * 128], psb(tb)[:, 0:512].rearrange("p (c t) -> p c t", c=4), ["ps%d" % tb], ["yaT.%d" % n])

        NIT = 2 * NT
        for t_ in range(NIT + 2):
            if t_ < NIT:
                at_sa(t_)
            if 0 <= t_ - 1 < NIT:
                at_sb(t_ - 1)
            if t_ >= 3 and (t_ - 1) % 2 == 0 and (t_ - 3) // 2 < NT:
                at_sc((t_ - 3) // 2)
        if "p3" in debug:
            d_ = dbg("yaT", [128, 4 * S], BF16)
            dma("sp", d_, yaT.rearrange("p c t -> p (c t)"), ["yaT.%d" % n for n in range(NT)], ["dbg_yaT"])
            outkeys.append("dbg_yaT")
            d_ = dbg("expb", [128, 3 * 8 * 128], F32)
            dma("sp", d_, expb.rearrange("p a b c -> p (a b c)"), ["expb"], ["dbg_expb"])
            outkeys.append("dbg_expb")
        P.barrier()
        A.release("tabs", "ohb", "mrow", "evec", "esink", "hk", "expb", "Es0", "Es1", "Pj0", "Pj1", "Pj2", "Pj3", "Pj4", "Pj5",
                  "yat0", "yat1", "dens", "qT", "kT", "vaug")


        GW = GW01 + [A.alloc("gw%d" % i, [S], F32) for i in range(2, 5)]
        ones1 = A.alloc("ones1", [1], F32)
        decs = A.alloc("decs", [16], F32)
        TS = A.alloc("TS", [3, NT, 36], F32)
        DECB = A.alloc("DECB", [8 * 16], F32)
        memset("dve", ones1, 1.0, ["ones1"])
        memset("dve", decs, 0.0, ["decs.0", "decs.1"])
        for i in range(2, 5):
            memset("dve", GW[i], 0.0, ["gw%d.0" % i, "gw%d.1" % i])
        def bk(kk):
            return ["gw%d.0" % kk, "gw%d.1" % kk]

        def both(kk):
            return GW[kk][0:36, :]
        act(both(1), both(1), AF.Exp, bk(1), bk(1), scale=-1.0)
        act(both(1), both(1), AF.Ln, bk(1), bk(1), bias=1.0)
        for d in range(2):
            p0 = 32 * d
            o_, i_ = GW[2][p0:p0 + 4, :], GW[1][p0:p0 + 4, :]
            if d == 1:
                o_, i_ = o_[:, ::-1], i_[:, ::-1]
            onesbc = ones1[p0:p0 + 4, 0:1].broadcast_to([4, S])
            P.op("dve", lambda e, o_=o_, i_=i_, ob=onesbc: e.tensor_tensor_scan(o_, ob, i_, 0.0, ALU.mult, ALU.subtract),
                 reads=["gw1.%d" % d, "ones1"], writes=["gw2.%d" % d])
        tt("dve", both(1), both(0), both(2), ALU.subtract, bk(0) + bk(2), bk(1))
        for d in range(2):
            p0 = 32 * d
            o_, i_ = GW[0][p0:p0 + 4, :], GW[1][p0:p0 + 4, :]
            if d == 1:
                o_, i_ = o_[:, ::-1], i_[:, ::-1]
            onesbc = ones1[p0:p0 + 4, 0:1].broadcast_to([4, S])
            P.op("dve", lambda e, o_=o_, i_=i_, ob=onesbc: e.tensor_tensor_scan(o_, ob, i_, 0.0, ALU.mult, ALU.max),
                 reads=["gw1.%d" % d, "ones1"], writes=["gw0.%d" % d])
            A3 = GW[0][p0:p0 + 4, :].rearrange("p (c t) -> p c t", c=NT)
            Mp3 = GW[3][p0:p0 + 4, :].rearrange("p (c t) -> p c t", c=NT)
            Mn3 = GW[4][p0:p0 + 4, :].rearrange("p (c t) -> p c t", c=NT)
            if d == 0:
                cp("dve", Mp3[:, 1:NT, :], A3[:, 0:NT - 1, 127:128].broadcast_to([4, NT - 1, 128]), ["gw0.0"], ["gw3.0"])
                cp("dve", Mn3, A3[:, :, 127:128].broadcast_to([4, NT, 128]), ["gw0.0"], ["gw4.0"])
            else:
                cp("dve", Mp3[:, 0:NT - 1, :], A3[:, 1:NT, 0:1].broadcast_to([4, NT - 1, 128]), ["gw0.1"], ["gw3.1"])
                cp("dve", Mn3, A3[:, :, 0:1].broadcast_to([4, NT, 128]), ["gw0.1"], ["gw4.1"])
        Mpb = both(3).rearrange("p (c t) -> p c t", c=NT)
        Mnb = both(4).rearrange("p (c t) -> p c t", c=NT)
        tt("dve", decs[0:36, :].unsqueeze(2), Mpb[:, :, 0:1], Mnb[:, :, 0:1], ALU.subtract, bk(3) + bk(4), ["decs.0", "decs.1"])
        act(decs[0:36, :], decs[0:36, :], AF.Exp, ["decs.0", "decs.1"], ["decs.0", "decs.1"])
        tt("dve", both(0), both(1), both(3), ALU.subtract, bk(1) + bk(3), bk(0))
        act(both(0), both(0), AF.Exp, bk(0), bk(0), bias=LNS)
        tt("dve", both(4), both(1), both(4), ALU.subtract, bk(1) + bk(4), bk(4))
        act(both(4), both(4), AF.Exp, bk(4), bk(4), bias=LNS)
        tt("dve", both(2), both(2), both(3), ALU.add, bk(2) + bk(3), bk(2))
        act(both(2), both(2), AF.Exp, bk(2), bk(2), scale=-1.0)
        if "p4pre" in debug:
            for i_ in range(5):
                d_ = dbg("gw%d" % i_, [128, S], F32)
                dma("sp", d_, GW[i_], ["gw%d.0" % i_, "gw%d.1" % i_], ["dbg_gw%d" % i_])
                outkeys.append("dbg_gw%d" % i_)
        checkpoint("p4pre")
        Esel = A.alloc("Esel", [8, 128], F32)
        for li, l_ in enumerate((0, 1, 2, 3, 32, 33, 34, 35)):
            cp("dve", Esel[0:36, li, :], identf[0:36, l_:l_ + 1].broadcast_to([36, 128]), ["identf"], ["Esel"])
        mmgroup([(psf(7)[:, li * 16:(li + 1) * 16], Esel[0:36, li, :], decs[0:36, :], True, True) for li in range(8)],
                ["Esel", "decs.0", "decs.1"], ["ps7"])
        cp("dve", DECB, psf(7)[:, 0:128], ["ps7"], ["DECB"])
        for ki, slot in enumerate((0, 4, 2)):
            for half in range(2):
                b = (ki * 2 + half) % 4
                trgroup([(psf(b)[:, ii * 36:(ii + 1) * 36], GW[slot][0:36, (half * 8 + ii) * 128:(half * 8 + ii + 1) * 128], identf[0:36, 0:36])
                         for ii in range(8)], ["gw%d.0" % slot, "gw%d.1" % slot, "identf"], ["ps%d" % b])
                cp("dve", TS[:, ki, half * 8:(half + 1) * 8, :], psf(b)[:, 0:288].rearrange("p (i c) -> p i c", i=8), ["ps%d" % b], ["TS"])
        if "p4a" in debug:
            d_ = dbg("TS", [128, 3 * NT * 36], F32)
            dma("sp", d_, TS.rearrange("p a b c -> p (a b c)"), ["TS"], ["dbg_TS"])
            outkeys.append("dbg_TS")
            d_ = dbg("DECB", [128, 8 * 16], F32)
            dma("sp", d_, DECB, ["DECB"], ["dbg_DECB"])
            outkeys.append("dbg_DECB")
        checkpoint("p4a")
        P.barrier()
        A.release("gw0", "gw1", "gw2", "gw3", "gw4", "ones1", "decs", "Esel")

        ktok = A.alloc("ktok", [NT, 4, 128], BF16)
        vu = A.alloc("vu", [2, NT, 4, 129], BF16)
        vu2 = A.alloc("vu2", [2, NT, 4, 129], BF16)
        for i in range(NT):
            b = i % 2
            trgroup([(psb(b)[:, h * 128:(h + 1) * 128], kmT[:, h, i * 128:(i + 1) * 128], identb) for h in range(4)],
                    ["kmT.%d" % h for h in range(4)] + ["identb"], ["ps%d" % b])
            cp("act", ktok[:, i, :, :], psb(b)[:, 0:512].rearrange("p (h d) -> p h d", h=4), ["ps%d" % b], ["ktok.%d" % i])
            for d in range(2):
                tt("dve", vu[:, d, i, :, :], vaugm[:, i, :, :], TS[:, 0, i, d * 32:d * 32 + 4].unsqueeze(2).broadcast_to([128, 4, 129]),
                   ALU.mult, ["vaugm.%d" % i, "vaugm1", "TS"], ["vu.%d.%d" % (d, i)])
                tt("dve", vu2[:, d, i, :, :], vaugm[:, i, :, :], TS[:, 1, i, d * 32:d * 32 + 4].unsqueeze(2).broadcast_to([128, 4, 129]),
                   ALU.mult, ["vaugm.%d" % i, "vaugm1", "TS"], ["vu2.%d.%d" % (d, i)])
        P.barrier()
        A.release("vaugm")
        Hparts = [A.alloc("Hh%d" % i, [NT // 2, 512], F32) for i in range(2)]

        def Hat(c_):
            return Hparts[c_ // (NT // 2)][:, c_ % (NT // 2), :]
        C32 = A.alloc("C32", [2, 4, 128], F32)
        n32 = A.alloc("n32", [2, 4], F32)
        Cbf = A.alloc("Cbf", [2, 4, 128], BF16)
        nbf = A.alloc("nbf", [2, 4], BF16)
        Smb = [A.alloc("Sm%d" % i, [4, 128], BF16) for i in range(2)]
        tmpH = [A.alloc("tmpH%d" % i, [4, 128], F32) for i in range(2)]
        tmp1 = A.alloc("tmp1", [2, 2, 4], F32)
        DECB3 = DECB.rearrange("p (l c) -> p l c", c=16)
        checkpoint("p4b")
        for step in range(NT):
            for d in range(2):
                c = step if d == 0 else NT - 1 - step
                cs = slice(c * 128, (c + 1) * 128)
                SA, OB, DB, CB = 4 * d, 4 * d + 1, 4 * d + 2, 4 * d + 3
                kSA, kOB, kDB, kCB = "ps%d" % SA, "ps%d" % OB, "ps%d" % DB, "ps%d" % CB
                qk_ = ["qmT.%d" % h for h in range(4)]
                mmgroup([(psf(SA)[:, h * 128:(h + 1) * 128], kmT[:, h, cs], qmT[:, h, cs], True, True) for h in range(4)],
                        ["kmT.%d" % h for h in range(4)] + qk_, [kSA])
                sm, smk = Smb[d], "Sm%d" % d
                msk = (maskf if d == 0 else maskb).unsqueeze(1).broadcast_to([128, 4, 128])
                tt("dve", sm, psf(SA).rearrange("p (h t) -> p h t", h=4), msk, ALU.mult, [kSA, "maskf", "maskb"], [smk])
                items, itemsd = [], []
                for h in range(4):
                    items.append((psf(OB)[:, h * 128:(h + 1) * 128], sm[:, h, :], vu[:, d, c, h, 0:128], True, step == 0))
                    if step > 0:
                        items.append((psf(OB)[:, h * 128:(h + 1) * 128], qmT[:, h, cs], Cbf[:, d, h, :], False, True))
                for h in range(4):
                    itemsd.append((psf(DB)[:, h:h + 1], sm[:, h, :], vu[:, d, c, h, 128:129], True, step == 0))
                    if step > 0:
                        itemsd.append((psf(DB)[:, h:h + 1], qmT[:, h, cs], nbf[:, d, h:h + 1], False, True))
                mmgroup(items, [smk, "vu.%d.%d" % (d, c)] + (qk_ + ["Cbf.%d" % d] if step > 0 else []), [kOB])
                mmgroup(itemsd, [smk, "vu.%d.%d" % (d, c)] + (qk_ + ["nbf.%d" % d] if step > 0 else []), [kDB])
                t1, rc = tmp1[:, d, 0, :], tmp1[:, d, 1, :]
                tk = "tmp1.%d" % d
                tt("dve", t1, psf(DB)[:, 0:4], TS[:, 2, c, d * 32:d * 32 + 4], ALU.max, [kDB, "TS"], [tk])
                stt(t1, psf(DB)[:, 0:4], -1.0, t1, ALU.mult, ALU.max, [kDB, tk], [tk])
                recip(rc, t1, [tk], [tk + "r"])
                hk_ = "H.%d" % c
                hdst = Hat(c).rearrange("p (h e) -> p h e", h=4)
                rcb = rc.unsqueeze(2).broadcast_to([128, 4, 128])
                first = (d == 0 and c < NT // 2) or (d == 1 and c >= NT // 2)
                if first:
                    tt("dve", hdst, psf(OB).rearrange("p (h e) -> p h e", h=4), rcb, ALU.mult, [kOB, tk + "r"], [hk_])
                else:
                    th, thk = tmpH[d], "tmpH%d" % d
                    tt("dve", th, psf(OB).rearrange("p (h e) -> p h e", h=4), rcb, ALU.mult, [kOB, tk + "r"], [thk])
                    tt("dve", hdst, hdst, th, ALU.add, [hk_, thk], [hk_])
                if step < NT - 1:
                    mmgroup([(psf(CB)[:, h * 128:(h + 1) * 128], ktok[:, c, h, :], vu2[:, d, c, h, 0:128], True, True) for h in range(4)],
                            ["ktok.%d" % c, "vu2.%d.%d" % (d, c)], [kCB])
                    mmgroup([(psf(DB)[:, 4 + h:5 + h], ktok[:, c, h, :], vu2[:, d, c, h, 128:129], True, True) for h in range(4)],
                            ["ktok.%d" % c, "vu2.%d.%d" % (d, c)], [kDB])
                    ck, nk = "C32.%d" % d, "n32.%d" % d
                    c32 = C32[:, d, :, :]
                    if step == 0:
                        cp("dve", c32, psf(CB).rearrange("p (h e) -> p h e", h=4), [kCB], [ck])
                        cp("dve", n32[:, d, :], psf(DB)[:, 4:8], [kDB], [nk])
                    else:
                        dec4 = DECB3[:, d * 4:d * 4 + 4, c]
                        tt("dve", c32, c32, dec4.unsqueeze(2).broadcast_to([128, 4, 128]), ALU.mult, [ck, "DECB"], [ck])
                        tt("dve", c32, c32, psf(CB).rearrange("p (h e) -> p h e", h=4), ALU.add, [ck, kCB], [ck])
                        tt("dve", n32[:, d, :], n32[:, d, :], dec4, ALU.mult, [nk, "DECB"], [nk])
                        tt("dve", n32[:, d, :], n32[:, d, :], psf(DB)[:, 4:8], ALU.add, [nk, kDB], [nk])
                    cp("act", Cbf[:, d, :, :], c32, [ck], ["Cbf.%d" % d])
                    cp("act", nbf[:, d, :], n32[:, d, :], [nk], ["nbf.%d" % d])
        if "p4" in debug:
            d_ = dbg("H", [128, NT * 512], F32)
            for hp_ in range(2):
                dma("sp", d_[:, hp_ * 4096:(hp_ + 1) * 4096], Hparts[hp_].rearrange("p a b -> p (a b)"), ["H.%d" % c for c in range(NT)], ["dbg_H%d" % hp_])
                outkeys.append("dbg_H%d" % hp_)
        checkpoint("p4c")
        P.barrier()
        A.release("ktok", "vu", "vu2", "C32", "n32", "Cbf", "nbf", "Sm0", "Sm1", "tmpH0", "tmpH1", "tmp1", "qmT", "kmT", "TS", "DECB")
        ymT = A.alloc("ymT", [4, S], BF16)
        ogb = A.alloc("ogb", [512], F32)
        sqh = A.alloc("sqh", [512], F32)
        tmph = A.alloc("tmph", [512], F32)
        sgt_all = A.alloc("sgtall", [NT, 512], F32)
        sgt = [sgt_all[:, i, :] for i in range(NT)]
        ymt = [A.alloc("ymt%d" % i, [512], BF16) for i in range(2)]
        ss4 = A.alloc("ss4", [4], F32)
        dma("sp", ogb, og_d.partition_broadcast(128), [], ["ogb"])
        for i in range(NT):
            dma("sp", sgt[i], sig_d[i * 128:(i + 1) * 128, :], ["sig_d.%d" % i], ["sgt%d" % i])
        for i in range(NT):
            hkeys = ["H.%d" % i]
            act(sqh, Hat(i), AF.Square, hkeys, ["sqh"])
            P.op("dve", lambda e: e.tensor_reduce(ss4, sqh.rearrange("p (h d) -> p h d", h=4), AX.X, ALU.add), reads=["sqh"], writes=["ss4"])
            act(ss4, ss4, AF.Sqrt, ["ss4"], ["ss4"], bias=EPS, scale=1.0 / 128)
            recip(ss4, ss4, ["ss4"], ["ss4"])
            tt("dve", tmph.rearrange("p (h d) -> p h d", h=4), Hat(i).rearrange("p (h d) -> p h d", h=4),
               ss4.unsqueeze(2).broadcast_to([128, 4, 128]), ALU.mult, hkeys + ["ss4"], ["tmph"])
            tt("dve", tmph, tmph, ogb, ALU.mult, ["tmph", "ogb"], ["tmph"])
            tt("dve", ymt[i % 2], tmph, sgt[i], ALU.mult, ["tmph", "sgt%d" % i], ["ymt%d" % (i % 2)])
            b = i % 2
            trgroup([(psb(b)[:, c * 128:(c + 1) * 128], ymt[i % 2][:, c * 128:(c + 1) * 128], identb) for c in range(4)],
                    ["ymt%d" % (i % 2), "identb"], ["ps%d" % b])
            cp("act", ymT[:, :, i * 128:(i + 1) * 128], psb(b)[:, 0:512].rearrange("p (c t) -> p c t", c=4), ["ps%d" % b], ["ymT.%d" % i])
        if "p4" in debug:
            d_ = dbg("ymT", [128, 4 * S], BF16)
            dma("sp", d_, ymT.rearrange("p c t -> p (c t)"), ["ymT.%d" % n for n in range(NT)], ["dbg_ymT"])
            outkeys.append("dbg_ymT")
        P.barrier()
        A.release("Hh0", "Hh1", "ogb", "sqh", "tmph", "sgtall", "ymt0", "ymt1", "ss4")


        hT2 = A.alloc("hT2", [8, S], BF16)
        yaT2 = yaT
        Wa = A.alloc("Wa", [4, D], BF16)
        Wm = A.alloc("Wm", [4, D], BF16)
        Wout = A.alloc("Wout", [8, D], BF16)
        wmg = [A.alloc("wmg%d" % i, [8, 2, 128], BF16) for i in range(2)]
        uT = A.alloc("uT", [8, S], BF16)
        gaS = [A.alloc("gaS%d" % i, [512], F32) for i in range(2)]
        gmS = [A.alloc("gmS%d" % i, [512], F32) for i in range(2)]
        u1S = [A.alloc("u1S%d" % i, [512], F32) for i in range(2)]
        u2S = [A.alloc("u2S%d" % i, [512], F32) for i in range(2)]
        dma("sp", hT2.rearrange("p c t -> p (c t)"), hT_d, ["hT_d"], ["hT2"])
        dma("pool", Wa, wview(wa_d), [], ["Wa"])
        dma("pool", Wm, wview(wm_d), [], ["Wm"])
        dma("pool", Wout, wview(wo_d), [], ["Wout"])
        ymkeys = ["ymT.%d" % n for n in range(NT)]
        mgsrc = wview(win_d)[:, :, 2832:4880].rearrange("p c (two f) -> p c two f", two=2)
        for fc in range(8):
            wg_, wgk = wmg[fc % 2], "wmg%d" % (fc % 2)
            for two in range(2):
                c0 = 2832 + two * 1024 + fc * 128
                dma("pool", wg_[:, :, two, :], wview(win_d)[:, :, c0:c0 + 128], [], [wgk + ".%d" % two])
            fs = slice(fc * 128, (fc + 1) * 128)
            for tc in range(4):
                it = fc * 4 + tc
                base = (it % 2) * 4
                tcs = slice(tc * 512, (tc + 1) * 512)
                mmgroup([(psf(base), Wa[:, kc, fs], yaT2[:, kc, tcs], kc == 0, kc == 3) for kc in range(4)], ["Wa"] + ["yaT.%d" % n_ for n_ in range(tc * 4, tc * 4 + 4)], ["ps%d" % base])
                mmgroup([(psf(base + 1), Wm[:, kc, fs], ymT[:, kc, tcs], kc == 0, kc == 3) for kc in range(4)],
                        ["Wm"] + ymkeys[tc * 4:tc * 4 + 4], ["ps%d" % (base + 1)])
                mmgroup([(psf(base + 2), wg_[:, c, 0, :], hT2[:, c, tcs], c == 0, c == 7) for c in range(8)], [wgk + ".0", "hT2"], ["ps%d" % (base + 2)])
                mmgroup([(psf(base + 3), wg_[:, c, 1, :], hT2[:, c, tcs], c == 0, c == 7) for c in range(8)], [wgk + ".1", "hT2"], ["ps%d" % (base + 3)])
                x2 = it % 2
                act(gaS[x2], psf(base + 2), AF.Sigmoid, ["ps%d" % (base + 2), "prm"], ["gaS%d" % x2], bias=prm[:, 48 + fc:49 + fc])
                act(gmS[x2], psf(base + 3), AF.Sigmoid, ["ps%d" % (base + 3), "prm"], ["gmS%d" % x2], bias=prm[:, 56 + fc:57 + fc])
                tt("dve", u1S[x2], gaS[x2], psf(base), ALU.mult, ["gaS%d" % x2, "ps%d" % base], ["u1S%d" % x2])
                tt("dve", u2S[x2], gmS[x2], psf(base + 1), ALU.mult, ["gmS%d" % x2, "ps%d" % (base + 1)], ["u2S%d" % x2])
                tt("dve", uT[:, fc, tcs], u1S[x2], u2S[x2], ALU.add, ["u1S%d" % x2, "u2S%d" % x2], ["uT.%d.%d" % (fc, tc)])
        if "p5" in debug:
            d_ = dbg("uT", [128, 8 * S], BF16)
            dma("sp", d_, uT.rearrange("p c t -> p (c t)"), ["uT.%d.%d" % (fc, tc) for fc in range(8) for tc in range(4)], ["dbg_uT"])
            outkeys.append("dbg_uT")
        checkpoint("p5")

        P.barrier()
        A.release("hT2", "yaT", "Wa", "Wm", "wmg0", "wmg1", "gaS0", "gaS1", "gmS0", "gmS1", "u1S0", "u1S1", "u2S0", "u2S1", "ymT")
        xts2 = [A.alloc("xts2_%d" % i, [D], F32) for i in range(2)]
        x1t = [A.alloc("x1t%d" % i, [D], F32) for i in range(2)]
        xnf = [A.alloc("xnf%d" % i, [D], F32) for i in range(2)]
        xnball = A.alloc("xnball", [NT, D], BF16)
        xnT = [A.alloc("xnT%d" % i, [8, 128], F32) for i in range(2)]
        junk2 = A.alloc("junk2", [D], BF16)
        Wr = A.alloc("Wr", [8, 36], F32)
        rbb = A.alloc("rbb", [36], F32)
        DST = A.alloc("DST", [NT, 2], I32)
        WTS = A.alloc("WTS", [NT, 2], F32)
        ss2 = A.alloc("ss2", [NT], F32)
        Lall = A.alloc("Lall", [NT, 36], F32)
        dma("sp", Wr[:, :, 0:4], wrg_d.rearrange("(c p) n -> p c n", p=128), [], ["Wr_a"])
        dma("sp", Wr[:, :, 4:36], wre_d.rearrange("(c p) n -> p c n", p=128), [], ["Wr_b"])
        dma("sp", rbb[:, 0:4], brg_d.partition_broadcast(128), [], ["rbb_a"])
        dma("sp", rbb[:, 4:36], bre_d.partition_broadcast(128), [], ["rbb_b"])
        uTk = lambda i: ["uT.%d.%d" % (fc, i // 4) for fc in range(8)]

        def r_s1(i):
            xt, xk = xts2[i % 2], "xts2_%d" % (i % 2)
            x1, x1k = x1t[i % 2], "x1t%d" % (i % 2)
            dma("sp", xt, x_d[i * 128:(i + 1) * 128, :], [], [xk])
            for half in range(2):
                b = 2 * (i % 2) + half
                hs = slice(half * 512, (half + 1) * 512)
                mmgroup([(psf(b), uT[:, c, i * 128:(i + 1) * 128], Wout[:, c, hs], c == 0, c == 7) for c in range(8)],
                        uTk(i) + ["Wout"], ["ps%d" % b])
                tt("dve", x1[:, hs], psf(b), xt[:, hs], ALU.add, ["ps%d" % b, xk], [x1k + ".%d" % half])
            x1ks = [x1k + ".0", x1k + ".1"]
            dma("sp", x1_d[i * 128:(i + 1) * 128, :], x1, x1ks, ["x1_d.%d" % i])
            act(junk2, x1, AF.Square, x1ks, ["junk2", "ss2.%d" % i], accum_out=ss2[:, i:i + 1])
            act(ss2[:, i:i + 1], ss2[:, i:i + 1], AF.Sqrt, ["ss2.%d" % i], ["ss2.%d" % i], bias=EPS, scale=1.0 / D)
            recip(ss2[:, i:i + 1], ss2[:, i:i + 1], ["ss2.%d" % i], ["ss2.%d" % i])
            stt(xnf[i % 2], x1, ss2[:, i:i + 1], g2b, ALU.mult, ALU.mult, x1ks + ["ss2.%d" % i, "g2b"], ["xnf%d" % (i % 2)])
            cp("act", xnball[:, i, :], xnf[i % 2], ["xnf%d" % (i % 2)], ["xnb.%d" % i])

        def r_s2(i):
            xf, xfk = xnf[i % 2], "xnf%d" % (i % 2)
            trgroup([(psf(4)[:, c * 128:(c + 1) * 128], xf[:, c * 128:(c + 1) * 128], identf) for c in range(4)], [xfk, "identf"], ["ps4"])
            trgroup([(psf(5)[:, c * 128:(c + 1) * 128], xf[:, (4 + c) * 128:(5 + c) * 128], identf) for c in range(4)], [xfk, "identf"], ["ps5"])
            cp("dve", xnT[i % 2][:, 0:4, :], psf(4).rearrange("p (c t) -> p c t", c=4), ["ps4"], ["xnT%d.a" % (i % 2)])
            cp("act", xnT[i % 2][:, 4:8, :], psf(5).rearrange("p (c t) -> p c t", c=4), ["ps5"], ["xnT%d.b" % (i % 2)])

        def r_s3(i):
            mmgroup([(psf(6)[:, 0:36], xnT[i % 2][:, c, :], Wr[:, c, :], c == 0, c == 7) for c in range(8)],
                    ["xnT%d.a" % (i % 2), "xnT%d.b" % (i % 2), "Wr_a", "Wr_b"], ["ps6"])
            tt("dve", Lall[:, i, :], psf(6)[:, 0:36], rbb, ALU.add, ["ps6", "rbb_a", "rbb_b"], ["Lall.%d" % i])

        for t_ in range(NT + 2):
            for off, stg in ((0, r_s1), (1, r_s2), (2, r_s3)):
                if 0 <= t_ - off < NT:
                    stg(t_ - off)
        Lk = ["Lall.%d" % i for i in range(NT)]
        Rs = A.alloc("Rs", [12, NT], F32)
        G4 = A.alloc("G4", [3, NT, 4], F32)
        B32 = A.alloc("B32", [8, NT, 32], F32)
        OHb = A.alloc("OHb", [NT, 32], BF16)
        gmax, sgs_, pg, v1, v2, e2, w1, w2 = (Rs[:, j, :] for j in range(8))
        d1f, d2f, ok1, ok2 = (Rs[:, j, :] for j in range(8, 12))
        ohg, pen, eg = G4[:, 0], G4[:, 1], G4[:, 2]
        em, em2, oh1, oh2, OHs, rin, dfull, tmp32 = (B32[:, j] for j in range(8))
        Lg = Lall[:, :, 0:4]
        Le = Lall[:, :, 4:36]

        def bc3(ap, n):
            return ap.unsqueeze(2).broadcast_to([128, NT, n])

        def red(out, in_, op, reads, writes):
            P.op("dve", lambda e: e.tensor_reduce(out, in_, AX.X, op), reads=reads, writes=writes)
        red(gmax, Lg, ALU.max, Lk, ["q.gmax"])
        tt("dve", ohg, Lg, bc3(gmax, 4), ALU.is_equal, Lk + ["q.gmax"], ["q.ohg"])
        tt("dve", eg, Lg, bc3(gmax, 4), ALU.subtract, Lk + ["q.gmax"], ["q.eg"])
        act(eg, eg, AF.Exp, ["q.eg"], ["q.eg"])
        red(sgs_, eg, ALU.add, ["q.eg"], ["q.sg"])
        recip(pg, sgs_, ["q.sg"], ["q.pg"])
        ts("dve", pen, ohg, -1.0, 1e30, ALU.add, ALU.mult, ["q.ohg"], ["q.pen"])
        tt("dve", em.rearrange("p i (g j) -> p i g j", g=4), Le.rearrange("p i (g j) -> p i g j", g=4),
           pen.unsqueeze(3).broadcast_to([128, NT, 4, 8]), ALU.add, Lk + ["q.pen"], ["q.em"])
        red(v1, em, ALU.max, ["q.em"], ["q.v1"])
        tt("dve", oh1, em, bc3(v1, 32), ALU.is_equal, ["q.em", "q.v1"], ["q.oh1"])
        stt(em2, oh1, -1e30, em, ALU.mult, ALU.add, ["q.oh1", "q.em"], ["q.em2"])
        red(v2, em2, ALU.max, ["q.em2"], ["q.v2"])
        tt("dve", oh2, em2, bc3(v2, 32), ALU.is_equal, ["q.em2", "q.v2"], ["q.oh2"])
        tt("dve", e2, v2, v1, ALU.subtract, ["q.v1", "q.v2"], ["q.e2"])
        act(e2, e2, AF.Exp, ["q.e2"], ["q.e2"])
        ts("dve", w1, e2, 1.0, None, ALU.add, None, ["q.e2"], ["q.w1"])
        recip(w1, w1, ["q.w1"], ["q.w1"])
        tt("dve", w1, w1, pg, ALU.mult, ["q.w1", "q.pg"], ["q.w1"])
        tt("dve", w2, w1, e2, ALU.mult, ["q.w1", "q.e2"], ["q.w2"])
        tt("dve", OHs, oh1, oh2, ALU.add, ["q.oh1", "q.oh2"], ["q.OH"])
        cp("dve", OHb, OHs, ["q.OH"], ["q.OHb"])
        items = []
        for i in range(NT):
            sl_ = psf(7)[:, i * 32:(i + 1) * 32]
            items.append((sl_, trib, OHb[:, i, :], True, i == 0))
            for j in range(i):
                items.append((sl_, onesb, OHb[:, j, :], False, j == i - 1))
        mmgroup(items, ["trib", "onesb", "q.OHb"], ["ps7"])
        cp("dve", rin, psf(7).rearrange("p (i e) -> p i e", i=NT), ["ps7"], ["q.rin"])
        tt("dve", dfull, rin, basee.unsqueeze(1).broadcast_to([128, NT, 32]), ALU.add, ["q.rin", "basee"], ["q.dfull"])
        ts("dve", rin, rin, float(CAPR), None, ALU.is_lt, None, ["q.rin"], ["q.okm"])
        ts("dve", dfull, dfull, -float(TRASH), None, ALU.add, None, ["q.dfull"], ["q.dfull"])
        tt("dve", dfull, dfull, rin, ALU.mult, ["q.dfull", "q.okm"], ["q.dfull"])
        ts("dve", dfull, dfull, float(TRASH), None, ALU.add, None, ["q.dfull"], ["q.dfull"])
        for (oh_, src_, dst_, kn) in ((oh1, dfull, d1f, "d1f"), (oh2, dfull, d2f, "d2f"), (oh1, rin, ok1, "ok1"), (oh2, rin, ok2, "ok2")):
            tt("dve", tmp32, oh_, src_, ALU.mult, ["q.oh1", "q.oh2", "q.dfull", "q.okm"], ["q.tmp32"])
            red(dst_, tmp32, ALU.add, ["q.tmp32"], ["q." + kn])
        tt("dve", WTS[:, :, 0], w1, ok1, ALU.mult, ["q.w1", "q.ok1"], ["WTS.a"])
        tt("dve", WTS[:, :, 1], w2, ok2, ALU.mult, ["q.w2", "q.ok2"], ["WTS.b"])
        cp("dve", DST[:, :, 0], d1f, ["q.d1f"], ["DST.a"])
        cp("dve", DST[:, :, 1], d2f, ["q.d2f"], ["DST.b"])
        for i in range(NT):
            for kk in range(2):
                P.op("pool", lambda e, o_=DST[:, i, kk:kk + 1], x_=xnball[:, i, :]: e.indirect_dma_start(
                    out=xs_d, out_offset=bass.IndirectOffsetOnAxis(ap=o_, axis=0), in_=x_, in_offset=None),
                    reads=["xnb.%d" % i, "DST.a", "DST.b", "xs"], writes=["xs.%d.%d" % (i, kk)], dma=True)
        xskeys = ["xs.%d.%d" % (i, kk) for i in range(NT) for kk in range(2)]
        if "p6a" in debug:
            d_ = dbg("DST", [128, NT * 2], I32)
            dma("sp", d_, DST.rearrange("p a b -> p (a b)"), ["DST.a", "DST.b"], ["dbg_DST"])
            outkeys.append("dbg_DST")
            d_ = dbg("WTS", [128, NT * 2], F32)
            dma("sp", d_, WTS.rearrange("p a b -> p (a b)"), ["WTS.a", "WTS.b"], ["dbg_WTS"])
            outkeys.append("dbg_WTS")
            d_ = dbg("x1", [S, D], F32)
            dma("sp", d_, x1_d, ["x1_d.%d" % i for i in range(NT)], ["dbg_x1"])
            outkeys.append("dbg_x1")
        checkpoint("p6a")
        P.barrier()
        A.release("Wout", "uT", "xts2_0", "xts2_1", "x1t0", "x1t1", "xnf0", "xnf1", "xnball", "xnT0", "xnT1", "junk2", "Wr", "rbb", "ss2",
                  "Lall", "Rs", "G4", "B32", "OHb")

        NWB = 4
        wgb = [A.alloc("wgb%d" % i, [8, 512], BF16) for i in range(NWB)]
        wub = [A.alloc("wub%d" % i, [8, 512], BF16) for i in range(NWB)]
        wdb = [A.alloc("wdb%d" % i, [4, D], BF16) for i in range(NWB)]
        xbr = [A.alloc("xbr%d" % i, [2, D], BF16) for i in range(3)]
        xbT = [A.alloc("xbT%d" % i, [8, CAPR], BF16) for i in range(3)]
        actT = [A.alloc("actT%d" % i, [4, CAPR], BF16) for i in range(2)]
        sgs = [A.alloc("sgs%d" % i, [CAPR], F32) for i in range(2)]
        ysb = [A.alloc("ysb%d" % i, [2, D], F32) for i in range(2)]
        NXB = 3

        def stageL(e_):
            s3, x3 = e_ % NWB, e_ % NXB
            dma("sp", xbr[x3], xs_d[e_ * CAPR:(e_ + 1) * CAPR, :].rearrange("(r p) d -> p r d", p=128), xskeys + ["xs"], ["xbr%d" % x3])
            dma("sp", wgb[s3], wgs_d[e_].rearrange("p (c n) -> p c n", c=8), ["wgs.%d" % e_], ["wgb%d" % s3])
            dma("sp", wub[s3], wus_d[e_].rearrange("p (c n) -> p c n", c=8), ["wus.%d" % e_], ["wub%d" % s3])
            dma("sp", wdb[s3], wds_d[e_].rearrange("p (c n) -> p c n", c=4), ["wds.%d" % e_], ["wdb%d" % s3])

        def stageT(e_):
            x3 = e_ % NXB
            for r in range(2):
                trgroup([(psb(r)[:, c * 128:(c + 1) * 128], xbr[x3][:, r, c::8], identb) for c in range(8)],
                        ["xbr%d" % x3, "identb"], ["ps%d" % r])
                cp("act" if r == 0 else "dve", xbT[x3][:, :, r * 128:(r + 1) * 128], psb(r).rearrange("p (c t) -> p c t", c=8),
                   ["ps%d" % r], ["xbT%d.%d" % (x3, r)])

        def stageB(e_):
            s3, x2, x3 = e_ % NWB, e_ % 2, e_ % NXB
            xtk = ["xbT%d.0" % x3, "xbT%d.1" % x3]
            for f in range(4):
                bg, bu = 2 + (f % 2) * 2, 3 + (f % 2) * 2
                fs = slice(f, 512, 4)
                mmgroup([(psf(bg)[:, 0:CAPR], wgb[s3][:, c, fs], xbT[x3][:, c, :], c == 0, c == 7) for c in range(8)], ["wgb%d" % s3] + xtk, ["ps%d" % bg])
                mmgroup([(psf(bu)[:, 0:CAPR], wub[s3][:, c, fs], xbT[x3][:, c, :], c == 0, c == 7) for c in range(8)], ["wub%d" % s3] + xtk, ["ps%d" % bu])
                act(sgs[f % 2], psf(bg)[:, 0:CAPR], AF.Silu, ["ps%d" % bg], ["sgs%d" % (f % 2)])
                tt("dve", actT[x2][:, f, :], sgs[f % 2], psf(bu)[:, 0:CAPR], ALU.mult, ["sgs%d" % (f % 2), "ps%d" % bu], ["actT%d.%d" % (x2, f)])

        def stageC(e_):
            s3, x2 = e_ % NWB, e_ % 2
            atk = ["actT%d.%d" % (x2, f) for f in range(4)]
            for r in range(2):
                for half in range(2):
                    b = 6 + (r * 2 + half) % 2
                    hs = slice(half * 512, (half + 1) * 512)
                    mmgroup([(psf(b), actT[x2][:, f, r * 128:(r + 1) * 128], wdb[s3][:, f, hs], f == 0, f == 3) for f in range(4)],
                            atk + ["wdb%d" % s3], ["ps%d" % b])
                    cp("act" if half == 0 else "dve", ysb[x2][:, r, hs], psf(b), ["ps%d" % b], ["ysb%d.%d.%d" % (x2, r, half)])
            dma("pool", ys_d[e_ * CAPR:(e_ + 1) * CAPR, :].rearrange("(r p) d -> p r d", p=128), ysb[x2],
                ["ysb%d.%d.%d" % (x2, r, half) for r in range(2) for half in range(2)], ["ys.%d" % e_])

        for t_ in range(NE + 3):
            for off, stg in ((0, stageL), (1, stageT), (2, stageB), (3, stageC)):
                if 0 <= t_ - off < NE:
                    stg(t_ - off)
        yskeys = ["ys.%d" % e_ for e_ in range(NE)] + ["ys_trash"]
        checkpoint("p6b")
        P.barrier()
        A.release(*["wgb%d" % i for i in range(NWB)], *["wub%d" % i for i in range(NWB)], *["wdb%d" % i for i in range(NWB)],
                  "xbr0", "xbr1", "xbr2", "xbT0", "xbT1", "xbT2", "actT0", "actT1", "sgs0", "sgs1", "ysb0", "ysb1")

        x1r = [A.alloc("x1r%d" % i, [D], F32) for i in range(4)]
        g1b_ = [A.alloc("gy1_%d" % i, [D], F32) for i in range(4)]
        g2b_ = [A.alloc("gy2_%d" % i, [D], F32) for i in range(4)]
        for i in range(NT):
            x2 = i % 4
            dma("sp", x1r[x2], x1_d[i * 128:(i + 1) * 128, :], ["x1_d.%d" % i], ["x1r%d" % x2])
            for kk, gb_ in enumerate((g1b_[x2], g2b_[x2])):
                P.op("pool", lambda e, o_=gb_, ix=DST[:, i, kk:kk + 1]: e.indirect_dma_start(
                    out=o_, out_offset=None, in_=ys_d, in_offset=bass.IndirectOffsetOnAxis(ap=ix, axis=0)),
                    reads=yskeys + ["DST.a", "DST.b"], writes=["gy%d_%d" % (kk + 1, x2)], dma=True)
            stt(x1r[x2], g1b_[x2], WTS[:, i, 0:1], x1r[x2], ALU.mult, ALU.add, ["gy1_%d" % x2, "WTS.a", "x1r%d" % x2], ["x1r%d" % x2])
            stt(x1r[x2], g2b_[x2], WTS[:, i, 1:2], x1r[x2], ALU.mult, ALU.add, ["gy2_%d" % x2, "WTS.b", "x1r%d" % x2], ["x1r%d" % x2])
            dma("sp", out_d[i * 128:(i + 1) * 128, :], x1r[x2], ["x1r%d" % x2], ["out.%d" % i])
            outkeys.append("out.%d" % i)

        STAGE = 6
        P.frozen = False
        P.op("sp", lambda e: None, reads=outkeys)
        stats = P.emit(st)
    return nc, stats, list(dbg_out.keys())


_CACHE = {}


def prep_inputs(inputs):
    f = lambda a: np.ascontiguousarray(np.asarray(a, dtype=np.float32))
    shared = dict(
        cst=make_consts(),
        norm1_g=f(inputs["norm1_g"][0]), w_in=f(inputs["w_in"][0]),
        attn_q_gain=f(inputs["attn_q_gain"][0]), attn_k_gain=f(inputs["attn_k_gain"][0]),
        attn_sink=f(inputs["attn_sink"][0]), rel_bias_table=f(inputs["rel_bias_table"]),
        mlstm_conv_w=f(inputs["mlstm_conv_w"][0]), mlstm_conv_b=f(inputs["mlstm_conv_b"][0]),
        mlstm_gate_b=f(inputs["mlstm_gate_b"][0]).reshape(16), mlstm_out_gain=f(inputs["mlstm_out_gain"][0]).reshape(512),
        w_branch_attn=f(inputs["w_branch_attn"][0]), w_branch_mlstm=f(inputs["w_branch_mlstm"][0]),
        merge_b=f(inputs["merge_b"][0]), w_out=f(inputs["w_out"][0]), norm2_g=f(inputs["norm2_g"][0]),
        w_router_group=f(inputs["w_router_group"][0]), b_router_group=f(inputs["b_router_group"][0]),
        w_router_expert=f(inputs["w_router_expert"][0]), b_router_expert=f(inputs["b_router_expert"][0]),
        w_expert_gate=f(inputs["w_expert_gate"][0]), w_expert_up=f(inputs["w_expert_up"][0]),
        w_expert_down=f(inputs["w_expert_down"][0]),
    )
    return shared


def kernel(**inputs):
    x = np.asarray(inputs["x"], dtype=np.float32)
    nb = x.shape[0]
    if "nc" not in _CACHE:
        _CACHE["nc"] = build_program()[0]
    nc = _CACHE["nc"]
    shared = prep_inputs(inputs)
    in_maps = []
    for b in range(nb):
        m = dict(shared)
        m["x"] = np.ascontiguousarray(x[b])
        in_maps.append(m)
    res = run_bass_kernel_spmd(nc, in_maps, core_ids=list(range(nb)))
    return np.stack([np.asarray(r["out"], dtype=np.float32) for r in res.results], axis=0)
```

```python
import math
import numpy as np
from contextlib import ExitStack
import concourse.bass as bass
import concourse.mybir as mybir
from concourse.bass_utils import run_bass_kernel_spmd

F32 = mybir.dt.float32
BF16 = mybir.dt.bfloat16
I32 = mybir.dt.int32
U8 = mybir.dt.uint8
AF = mybir.ActivationFunctionType
ALU = mybir.AluOpType
AX = mybir.AxisListType

S = 2048
D = 1024
NT = 16
DIN = 4880
NE = 32
CAPR = 256
PRECAST = [e for e in range(32) if e % 8 >= 3]
NROWS = NE * CAPR + 128
TRASH = NE * CAPR
EPS = 1e-6
LNS = math.log(128.0 ** -0.5)
ENGS = ("pe", "act", "dve", "pool", "sp")

C_ID, C_J, C_MF, C_MB, C_TRI, C_ONE, C_IOTA, C_OHB, C_MROW, NCST = 0, 128, 256, 384, 512, 640, 768, 800, 1312, 1824


class Op:
    __slots__ = ("eng", "fn", "reads", "writes", "dma", "idx", "deps", "needs_inc", "incval", "dsem", "dval", "barrier", "bg")

    def __init__(self, eng, fn, reads, writes, dma):
        self.eng, self.fn, self.reads, self.writes, self.dma = eng, fn, tuple(reads), tuple(writes), dma
        self.deps = []
        self.needs_inc = False
        self.incval = 0
        self.dsem = None
        self.dval = 0
        self.barrier = False
        self.bg = False


class Prog:
    NDS = 56

    def __init__(self, nc):
        self.nc = nc
        self.ops = []

    frozen = False

    def op(self, eng, fn, reads=(), writes=(), dma=False):
        o = Op(eng, fn, reads, writes, dma)
        if not self.frozen:
            self.ops.append(o)
        return o

    def barrier(self):
        if self.frozen:
            return
        for e in ENGS:
            o = Op(e, None, (), (), False)
            o.barrier = True
            self.ops.append(o)

    def emit(self, stack):
        nc = self.nc
        engobj = {"pe": nc.tensor, "act": nc.scalar, "dve": nc.vector, "pool": nc.gpsimd, "sp": nc.sync}
        ops = self.ops
        lastw, readers = {}, {}
        last_on = {e: None for e in ENGS}
        all_dma = []
        for i, o in enumerate(ops):
            o.idx = i
            deps = set()
            rawset = set()
            if o.barrier:
                for e in ENGS:
                    if e != o.eng and last_on[e] is not None:
                        deps.add(last_on[e])
                deps.update(d_ for d_ in all_dma if not ops[d_].bg)
            else:
                for k in o.reads:
                    w = lastw.get(k)
                    if w is not None:
                        deps.add(w)
                        rawset.add(w)
                    if k.startswith("ps"):
                        deps.update(readers.get(k, ()))
                for k in o.writes:
                    w = lastw.get(k)
                    if w is not None:
                        deps.add(w)
                    deps.update(readers.get(k, ()))
            fdeps = []
            for j in deps:
                p = ops[j]
                if (not p.dma) and (not o.dma) and p.eng == o.eng:
                    if j not in rawset or o.eng == "pe":
                        continue
                fdeps.append(j)
            o.deps = fdeps
            if not o.barrier:
                for k in o.reads:
                    readers.setdefault(k, []).append(i)
                for k in o.writes:
                    lastw[k] = i
                    readers[k] = []
            if o.dma:
                all_dma.append(i)
            elif not o.barrier:
                last_on[o.eng] = i
            if o.barrier and o.eng == ENGS[-1]:
                all_dma = []
        for o in ops:
            for j in o.deps:
                if not ops[j].dma:
                    ops[j].needs_inc = True
        cnt = {e: 0 for e in ENGS}
        for o in ops:
            if o.dma or o.barrier:
                continue
            if o.needs_inc:
                cnt[o.eng] += 1
            o.incval = cnt[o.eng]
        esem = {e: stack.enter_context(nc.semaphore("s_" + e)) for e in ENGS}
        dsems = [stack.enter_context(nc.semaphore("d%d" % i)) for i in range(self.NDS)]
        duse = [0] * self.NDS
        nd = 0
        nd_sw = 0
        nd_hw = 0
        seen = {e: {f: 0 for f in ENGS} for e in ENGS}
        dseen = {e: {} for e in ENGS}
        nwait = 0
        for o in ops:
            e = engobj[o.eng]
            waits, dwaits = {}, {}
            for j in o.deps:
                p = ops[j]
                if p.dma:
                    if dseen[o.eng].get(p.dsem, 0) < p.dval:
                        dwaits[p.dsem] = max(dwaits.get(p.dsem, 0), p.dval)
                else:
                    v = p.incval
                    if seen[o.eng][p.eng] < v:
                        waits[p.eng] = max(waits.get(p.eng, 0), v)
            if o.dma:
                half = self.NDS // 2
                if o.eng == "pool":
                    s = nd_sw % half
                    nd_sw += 1
                else:
                    s = half + (nd_hw % half)
                    nd_hw += 1
                nd += 1
                if duse[s] > 0:
                    pv = duse[s] * 16
                    if dseen[o.eng].get(s, 0) < pv:
                        dwaits[s] = max(dwaits.get(s, 0), pv)
                duse[s] += 1
                o.dsem = s
                o.dval = duse[s] * 16
            for f, v in waits.items():
                e.wait_ge(esem[f], v)
                seen[o.eng][f] = v
                nwait += 1
            for s, v in dwaits.items():
                e.wait_ge(dsems[s], v)
                dseen[o.eng][s] = v
                nwait += 1
            if o.barrier:
                continue
            ins = o.fn(e)
            if o.dma:
                ins.then_inc(dsems[o.dsem], 16)
            elif o.needs_inc:
                ins.then_inc(esem[o.eng], 1)
        self.stats = dict(n_ops=len(ops), n_wait=nwait, incs=dict(cnt), n_dma=nd)
        return self.stats


class Arena:
    def __init__(self, nc, stack, nbytes):
        self.t = stack.enter_context(nc.sbuf_tensor("arena", [128, nbytes], U8))
        self.free = [(0, nbytes)]
        self.live = {}

    def alloc(self, name, shape, dt):
        esz = {F32: 4, BF16: 2, I32: 4}[dt]
        n = int(np.prod(shape)) * esz
        n_al = (n + 63) // 64 * 64
        for idx, (off, sz) in enumerate(self.free):
            if sz >= n_al:
                self.free[idx] = (off + n_al, sz - n_al)
                if self.free[idx][1] == 0:
                    del self.free[idx]
                break
        else:
            raise RuntimeError("arena OOM for %s (%d bytes); free=%s" % (name, n_al, self.free))
        assert name not in self.live, name
        self.live[name] = (off, n_al)
        v = self.t[:, off:off + n].bitcast(dt)
        if len(shape) == 2:
            v = v.rearrange("p (a b) -> p a b", a=shape[0])
        elif len(shape) == 3:
            v = v.rearrange("p (a b c) -> p a b c", a=shape[0], b=shape[1])
        elif len(shape) == 4:
            v = v.rearrange("p (a b c d) -> p a b c d", a=shape[0], b=shape[1], c=shape[2])
        return v

    def release(self, *names):
        for name in names:
            off, sz = self.live.pop(name)
            self.free.append((off, sz))
        self.free.sort()
        m = []
        for off, sz in self.free:
            if m and m[-1][0] + m[-1][1] == off:
                m[-1] = (m[-1][0], m[-1][1] + sz)
            else:
                m.append((off, sz))
        self.free = m


def t5_bucket_np(rel):
    half, max_exact = 16, 8
    sign = np.where(rel > 0, half, 0)
    n = np.abs(rel)
    nf = np.maximum(n, 1).astype(np.float32)
    large = max_exact + (np.log(nf / max_exact) / math.log(128 / max_exact) * (half - max_exact)).astype(np.int32)
    large = np.minimum(large, half - 1)
    return sign + np.where(n < max_exact, n, large)


def make_consts():
    c = np.zeros((128, NCST), np.float32)
    p = np.arange(128)[:, None]
    q = np.arange(128)[None, :]
    c[:, C_ID:C_ID + 128] = (p == q)
    c[:, C_J:C_J + 128] = (p + q == 127)
    c[:, C_MF:C_MF + 128] = (p <= q)
    c[:, C_MB:C_MB + 128] = (p >= q)
    c[:, C_TRI:C_TRI + 128] = (p < q)
    c[:, C_ONE:C_ONE + 128] = 1.0
    c[:, C_IOTA:C_IOTA + 32] = np.arange(32)[None, :] * float(CAPR)
    r = np.arange(512)
    rel = r - 255
    valid = np.abs(rel) <= 128
    bk = t5_bucket_np(rel)
    for b in range(32):
        c[b, C_OHB:C_OHB + 512] = ((bk == b) & valid).astype(np.float32)
    c[0:8, C_MROW:C_MROW + 512] = np.where(valid, 0.0, -30000.0)[None, :]
    return c


def build_program(debug=(), stop=None):
    nc = bass.Bass("TRN2", target_bir_lowering=False)

    def din(name, shape, dt=F32):
        return nc.dram_tensor(name, list(shape), dt, kind="ExternalInput").ap()

    def dscr(name, shape, dt):
        return nc.dram_tensor(name, list(shape), dt, kind="Internal").ap()

    x_d = din("x", [S, D])
    cst_d = din("cst", [128, NCST])
    g1_d = din("norm1_g", [D])
    win_d = din("w_in", [D, DIN])
    gq_d = din("attn_q_gain", [64])
    gk_d = din("attn_k_gain", [64])
    sink_d = din("attn_sink", [8])
    tab_d = din("rel_bias_table", [32, 8])
    cw_d = din("mlstm_conv_w", [5, 1024])
    cb_d = din("mlstm_conv_b", [1024])
    gb_d = din("mlstm_gate_b", [16])
    og_d = din("mlstm_out_gain", [512])
    wa_d = din("w_branch_attn", [512, D])
    wm_d = din("w_branch_mlstm", [512, D])
    mb_d = din("merge_b", [2048])
    wo_d = din("w_out", [D, D])
    g2_d = din("norm2_g", [D])
    wrg_d = din("w_router_group", [D, 4])
    brg_d = din("b_router_group", [4])
    wre_d = din("w_router_expert", [D, 32])
    bre_d = din("b_router_expert", [32])
    weg_d = din("w_expert_gate", [NE, D, 512])
    weu_d = din("w_expert_up", [NE, D, 512])
    wed_d = din("w_expert_down", [NE, 512, D])
    out_d = nc.dram_tensor("out", [S, D], F32, kind="ExternalOutput").ap()

    hT_d = dscr("hT_scr", [128, 8 * S], BF16)
    sig_d = dscr("sig_scr", [S, 512], F32)
    yaT_d = dscr("yaT_scr", [128, 4 * S], BF16)
    x1_d = dscr("x1_scr", [S, D], F32)
    xs_d = dscr("xs_scr", [NROWS, D], BF16)
    ys_d = dscr("ys_scr", [NROWS, D], F32)
    dec_d = dscr("dec_scr", [36 * 16], F32)
    wgs_d = dscr("wgs_scr", [NE, 128, 4096], BF16)
    wus_d = dscr("wus_scr", [NE, 128, 4096], BF16)
    wds_d = dscr("wds_scr", [NE, 128, 4096], BF16)
    ev_d = dscr("ev_scr", [8, 512], F32)

    dbg_out = {}

    def dbg(name, shape, dt=F32):
        dbg_out[name] = nc.dram_tensor("dbg_" + name, list(shape), dt, kind="ExternalOutput").ap()
        return dbg_out[name]

    P = Prog(nc)
    outkeys = []

    def checkpoint(name):
        if stop == name:
            P.frozen = True
    with ExitStack() as st:
        A = Arena(nc, st, 204 * 1024)
        PS = [st.enter_context(nc.psum_tensor("ps%d" % b, [128, 512], F32)) for b in range(8)]

        def psf(b):
            return PS[b][:]

        def psb(b):
            return PS[b][:].bitcast(BF16)

        def dma(q, out, in_, reads, writes):
            P.op(q, lambda e: e.dma_start(out=out, in_=in_), reads=reads, writes=writes, dma=True)

        def mmgroup(items, reads, writes):
            def fn(e):
                ins = None
                for (o_, l_, r_, s0, s1) in items:
                    ins = e.matmul(o_, l_, r_, start=s0, stop=s1)
                return ins
            P.op("pe", fn, reads=reads, writes=writes)

        def trgroup(items, reads, writes):
            def fn(e):
                ins = None
                for (o_, i_, id_) in items:
                    ins = e.transpose(o_, i_, id_)
                return ins
            P.op("pe", fn, reads=reads, writes=writes)

        def act(out, in_, func, reads, writes, bias=0.0, scale=1.0, accum_out=None):
            if accum_out is None:
                P.op("act", lambda e: e.activation(out=out, in_=in_, func=func, bias=bias, scale=scale), reads=reads, writes=writes)
            else:
                P.op("act", lambda e: e.activation(out=out, in_=in_, func=func, bias=bias, scale=scale, accum_out=accum_out), reads=reads, writes=writes)

        def tt(eng, out, in0, in1, op, reads, writes):
            P.op(eng, lambda e: e.tensor_tensor(out, in0, in1, op), reads=reads, writes=writes)

        def ts(eng, out, in0, s1, s2, op0, op1, reads, writes):
            if op1 is None:
                P.op(eng, lambda e: e.tensor_scalar(out, in0, s1, None, op0), reads=reads, writes=writes)
            else:
                P.op(eng, lambda e: e.tensor_scalar(out, in0, s1, s2, op0, op1), reads=reads, writes=writes)

        def stt(out, in0, scalar, in1, op0, op1, reads, writes, accum_out=None):
            if accum_out is None:
                P.op("dve", lambda e: e.scalar_tensor_tensor(out, in0, scalar, in1, op0, op1), reads=reads, writes=writes)
            else:
                P.op("dve", lambda e: e.scalar_tensor_tensor(out, in0, scalar, in1, op0, op1, accum_out=accum_out), reads=reads, writes=writes)

        def cp(eng, out, in_, reads, writes):
            if eng == "act":
                P.op("act", lambda e: e.activation(out=out, in_=in_, func=AF.Copy), reads=reads, writes=writes)
            else:
                P.op(eng, lambda e: e.tensor_copy(out, in_), reads=reads, writes=writes)

        def recip(out, in_, reads, writes):
            P.op("dve", lambda e: e.reciprocal(out, in_), reads=reads, writes=writes)

        def memset(eng, ap, val, writes):
            P.op(eng, lambda e: e.memset(ap, val), writes=writes)

        def wview(w_d):
            return w_d.rearrange("(c p) n -> p c n", p=128)

        identf = A.alloc("identf", [128], F32)
        Jf = A.alloc("Jf", [128], F32)
        maskf = A.alloc("maskf", [128], F32)
        maskb = A.alloc("maskb", [128], F32)
        identb = A.alloc("identb", [128], BF16)
        trib = A.alloc("trib", [128], BF16)
        onesb = A.alloc("onesb", [128], BF16)
        basee = A.alloc("basee", [32], F32)
        g1b = A.alloc("g1b", [D], F32)
        g2b = A.alloc("g2b", [D], F32)
        prm = A.alloc("prm", [64], F32)
        gbc = A.alloc("gbc", [2], F32)
        dma("sp", identf, cst_d[:, C_ID:C_ID + 128], [], ["identf"])
        dma("sp", Jf, cst_d[:, C_J:C_J + 128], [], ["Jf"])
        dma("sp", maskf, cst_d[:, C_MF:C_MF + 128], [], ["maskf"])
        dma("sp", maskb, cst_d[:, C_MB:C_MB + 128], [], ["maskb"])
        dma("sp", basee, cst_d[:, C_IOTA:C_IOTA + 32], [], ["basee"])
        dma("pool", identb, cst_d[:, C_ID:C_ID + 128], [], ["identb"])
        dma("pool", trib, cst_d[:, C_TRI:C_TRI + 128], [], ["trib"])
        dma("pool", onesb, cst_d[:, C_ONE:C_ONE + 128], [], ["onesb"])
        dma("sp", g1b, g1_d.partition_broadcast(128), [], ["g1b"])
        dma("sp", g2b, g2_d.partition_broadcast(128), [], ["g2b"])
        memset("dve", gbc, 0.0, ["gbc"])
        for d_ in range(2):
            for f_ in range(2):
                dma("sp", gbc[32 * d_:32 * d_ + 4, f_:f_ + 1], bass.AP(gb_d.tensor, d_ * 8 + f_ * 4, [[1, 4], [1, 1]]), ["gbc"], ["gbc"])
        zt = A.alloc("zt", [D], F32)
        memset("dve", zt, 0.0, ["zt"])
        ztb = zt.bitcast(BF16)[:, 0:D]
        pst = A.alloc("pst", [128], F32)
        dma("sp", pst[0:40, :], cw_d.rearrange("j (g p) -> (j g) p", p=128), [], ["pst_a"])
        dma("sp", pst[40:48, :], cb_d.rearrange("(g p) -> g p", p=128), [], ["pst_b"])
        dma("sp", pst[48:64, :], mb_d.rearrange("(g p) -> g p", p=128), [], ["pst_c"])
        trgroup([(psf(0)[:, 0:64], pst[0:64, :], identf[0:64, 0:64])], ["pst_a", "pst_b", "pst_c", "identf"], ["ps0"])
        cp("dve", prm, psf(0)[:, 0:64], ["ps0"], ["prm"])

        hT = A.alloc("hT", [8, S], BF16)
        xts = [A.alloc("xt%d" % i, [D], F32) for i in range(2)]
        hbfs = [A.alloc("hbf%d" % i, [D], BF16) for i in range(2)]
        junk = A.alloc("junk", [D], BF16)
        ss1 = A.alloc("ss1", [NT], F32)
        rs1 = A.alloc("rs1", [NT], F32)
        for i in range(NT):
            xt, hb = xts[i % 2], hbfs[i % 2]
            kx, kh = "xt%d" % (i % 2), "hbf%d" % (i % 2)
            dma("sp", xt, x_d[i * 128:(i + 1) * 128, :], [], [kx])
            act(junk, xt, AF.Square, [kx], ["junk", "ss1.%d" % i], accum_out=ss1[:, i:i + 1])
            act(rs1[:, i:i + 1], ss1[:, i:i + 1], AF.Sqrt, ["ss1.%d" % i], ["rs1.%d" % i], bias=EPS, scale=1.0 / D)
            recip(rs1[:, i:i + 1], rs1[:, i:i + 1], ["rs1.%d" % i], ["rs1.%d" % i])
            stt(hb, xt, rs1[:, i:i + 1], g1b, ALU.mult, ALU.mult, [kx, "rs1.%d" % i, "g1b"], [kh])
            b = i % 2
            trgroup([(psb(b)[:, c * 128:(c + 1) * 128], hb[:, c * 128:(c + 1) * 128], identb) for c in range(8)],
                    [kh, "identb"], ["ps%d" % b])
            cp("act" if i % 2 else "dve", hT[:, :, i * 128:(i + 1) * 128],
               psb(b).rearrange("p (c t) -> p c t", c=8), ["ps%d" % b], ["hT.%d" % i])
        P.barrier()
        A.release("xt0", "xt1", "hbf0", "hbf1", "junk", "ss1", "rs1", "pst")
        if "hT" in debug:
            d_ = dbg("hT", [128, 8 * S], BF16)
            dma("sp", d_, hT.rearrange("p c t -> p (c t)"), ["hT.%d" % i for i in range(NT)], ["dbg_hT"])
            outkeys.append("dbg_hT")
        hT_keys = ["hT.%d" % i for i in range(NT)]

        def hT_tc_keys(tc):
            return ["hT.%d" % i for i in range(tc * 4, tc * 4 + 4)]

        dma("sp", xs_d.rearrange("(r p) d -> p r d", p=128),
            ztb.unsqueeze(1).broadcast_to([128, NROWS // 128, D]), ["zt"], ["xs"])
        dma("sp", ys_d[TRASH:TRASH + 128, :], zt, ["zt"], ["ys_trash"])
        wbuf = [A.alloc("wbuf%d" % i, [8, 512], BF16) for i in range(2)]
        wcnt = [0]

        def load_w(c0, ncols):
            i = wcnt[0] % 2
            wcnt[0] += 1
            dma("pool", wbuf[i][:, :, 0:ncols], wview(win_d)[:, :, c0:c0 + ncols], [], ["wbuf%d" % i])
            return wbuf[i], "wbuf%d" % i

        qT = A.alloc("qT", [4, S], BF16)
        kT = A.alloc("kT", [S], BF16)
        vaug = A.alloc("vaug", [NT, 2, 65], BF16)
        qmT = A.alloc("qmT", [4, S], BF16)
        kmT = A.alloc("kmT", [4, S], BF16)
        vaugm = A.alloc("vaugm", [NT, 4, 129], BF16)
        GW01 = [A.alloc("gw%d" % i, [S], F32) for i in range(2)]
        gqb = A.alloc("gqb", [64], F32)
        gkb = A.alloc("gkb", [64], F32)
        sq = A.alloc("sq", [512], F32)
        tmpq = A.alloc("tmpq", [512], F32)
        qns = [A.alloc("qn%d" % i, [512], BF16) for i in range(2)]
        ssq = A.alloc("ssq", [8], F32)
        sgo = [A.alloc("sgo%d" % i, [512], F32) for i in range(2)]
        dma("sp", gqb, gq_d.partition_broadcast(128), [], ["gqb"])
        dma("sp", gkb, gk_d.partition_broadcast(128), [], ["gkb"])
        act(gqb, gqb, AF.Copy, ["gqb"], ["gqb"], scale=0.125)
        memset("dve", vaug[:, :, :, 64:65], 1.0, ["vaug1"])
        memset("dve", vaugm[:, :, :, 128:129], 1.0, ["vaugm1"])

        def tokmajor(c0, ncols, bank0, consume, consume_b=None):
            w, wk = load_w(c0, ncols)
            for i in range(NT):
                b = i % 4
                mmgroup([(psf(b)[:, 0:ncols], hT[:, c, i * 128:(i + 1) * 128], w[:, c, 0:ncols], c == 0, c == 7) for c in range(8)],
                        ["hT.%d" % i, wk], ["ps%d" % b])
                consume(i, b)
                if consume_b is not None and i >= 1:
                    consume_b(i - 1, (i - 1) % 4)
            if consume_b is not None:
                consume_b(NT - 1, (NT - 1) % 4)

        def qk_norm(src, nh, gain, gk_, out_bf, reads, tag):
            act(sq[:, 0:nh * 64], src, AF.Square, reads, ["sq"])
            P.op("dve", lambda e: e.tensor_reduce(ssq[:, 0:nh], sq[:, 0:nh * 64].rearrange("p (h d) -> p h d", h=nh), AX.X, ALU.add),
                 reads=["sq"], writes=["ssq"])
            act(ssq[:, 0:nh], ssq[:, 0:nh], AF.Sqrt, ["ssq"], ["ssq"], bias=EPS, scale=1.0 / 64)
            recip(ssq[:, 0:nh], ssq[:, 0:nh], ["ssq"], ["ssq"])
            tt("dve", tmpq[:, 0:nh * 64].rearrange("p (h d) -> p h d", h=nh), src.rearrange("p (h d) -> p h d", h=nh),
               ssq[:, 0:nh].unsqueeze(2).broadcast_to([128, nh, 64]), ALU.mult, reads + ["ssq"], ["tmpq"])
            if nh == 8:
                tt("dve", out_bf.rearrange("p (a g d) -> p g a d", a=4, g=2), tmpq.rearrange("p (g a d) -> p g a d", g=2, a=4),
                   gain.unsqueeze(1).unsqueeze(1).broadcast_to([128, 2, 4, 64]), ALU.mult, ["tmpq", gk_], [tag])
            else:
                tt("dve", out_bf.rearrange("p (h d) -> p h d", h=nh), tmpq[:, 0:nh * 64].rearrange("p (h d) -> p h d", h=nh),
                   gain.unsqueeze(1).broadcast_to([128, nh, 64]), ALU.mult, ["tmpq", gk_], [tag])

        def consume_q(i, b):
            qk_norm(psf(b), 8, gqb, "gqb", qns[i % 2], ["ps%d" % b], "qn%d" % (i % 2))

        def consume_q_b(i, b):
            tb = 4 + (i % 2)
            q_, qk_ = qns[i % 2], "qn%d" % (i % 2)
            trgroup([(psb(tb)[:, a * 128:(a + 1) * 128], q_[:, a * 128:(a + 1) * 128], identb) for a in range(4)], [qk_, "identb"], ["ps%d" % tb])
            cp("act", qT[:, :, i * 128:(i + 1) * 128], psb(tb)[:, 0:512].rearrange("p (a t) -> p a t", a=4), ["ps%d" % tb], ["qT.%d" % i])

        def consume_kv(i, b):
            qk_norm(psf(b)[:, 0:128], 2, gkb, "gkb", qns[i % 2][:, 0:128], ["ps%d" % b], "qn%d" % (i % 2))
            cp("act", vaug[:, i, :, 0:64], psf(b)[:, 128:256].rearrange("p (g d) -> p g d", g=2), ["ps%d" % b], ["vaug.%d" % i])

        def consume_kv_b(i, b):
            tb = 4 + (i % 2)
            q_, qk_ = qns[i % 2], "qn%d" % (i % 2)
            trgroup([(psb(tb)[:, 0:128], q_[:, 0:128], identb)], [qk_, "identb"], ["ps%d" % tb])
            cp("act", kT[:, i * 128:(i + 1) * 128], psb(tb)[:, 0:128], ["ps%d" % tb], ["kT.%d" % i])

        def consume_vm(i, b):
            cp("act" if i % 2 else "dve", vaugm[:, i, :, 0:128], psf(b).rearrange("p (h d) -> p h d", h=4), ["ps%d" % b], ["vaugm.%d" % i])

        def consume_om(i, b):
            sg_, sk = sgo[i % 2], "sgo%d" % (i % 2)
            act(sg_, psf(b), AF.Sigmoid, ["ps%d" % b], [sk])
            dma("sp", sig_d[i * 128:(i + 1) * 128, :], sg_, [sk], ["sig_d.%d" % i])

        tokmajor(0, 512, 0, consume_q, consume_q_b)
        tokmajor(512, 256, 2, consume_kv, consume_kv_b)
        tokmajor(1792, 512, 0, consume_vm)
        tokmajor(2304, 512, 2, consume_om)

        xpb = [A.alloc("xpb%d" % i, [S + 4], BF16) for i in range(2)]
        dg = A.alloc("dg", [8, 5, 128], BF16)
        for g in range(8):
            for j in range(5):
                ts("pool", dg[:, g, j, :], identb, prm[:, j * 8 + g:j * 8 + g + 1], None, ALU.mult, None, ["identb", "prm"], ["dg.%d" % g])
        for i_ in range(2):
            memset("dve", xpb[i_][:, 0:2], 0.0, ["xpb%d_l" % i_])
            memset("dve", xpb[i_][:, S + 2:S + 4], 0.0, ["xpb%d_r" % i_])
        wqk = {}

        def cv_proj(g):
            qk, h = g // 4, g % 4
            if h == 0:
                wqk[qk] = load_w(768 + 512 * qk, 512)
            w, wk = wqk[qk]
            xb_ = xpb[g % 2]
            for tc in range(4):
                b = 6 + (tc % 2)
                mmgroup([(psf(b), w[:, c, h * 128:(h + 1) * 128], hT[:, c, tc * 512:(tc + 1) * 512], c == 0, c == 7) for c in range(8)],
                        hT_tc_keys(tc) + [wk], ["ps%d" % b])
                cp("act", xb_[:, 2 + tc * 512:2 + (tc + 1) * 512], psf(b), ["ps%d" % b], ["xpb%d.%d" % (g % 2, tc)])

        def cv_conv(g):
            qk, h = g // 4, g % 4
            dst = qmT if qk == 0 else kmT
            xb_ = xpb[g % 2]
            xk = ["xpb%d.%d" % (g % 2, t) for t in range(4)] + ["xpb%d_l" % (g % 2), "xpb%d_r" % (g % 2)]
            for tc in range(4):
                b = 4 + (tc % 2)
                mmgroup([(psf(b), dg[:, g, j, :], xb_[:, tc * 512 + j:tc * 512 + j + 512], j == 0, j == 4) for j in range(5)],
                        xk + ["dg.%d" % g], ["ps%d" % b])
                act(dst[:, h, tc * 512:(tc + 1) * 512], psf(b), AF.Silu, ["ps%d" % b, "prm"],
                    ["%s.%d" % ("qmT" if qk == 0 else "kmT", h)], bias=prm[:, 40 + g:41 + g])

        for g in range(9):
            if g < 8:
                cv_proj(g)
            if g >= 1:
                cv_conv(g - 1)
        w, wk = load_w(2816, 16)
        wpad = A.alloc("wpad", [8, 2, 36], BF16)
        memset("dve", wpad, 0.0, ["wpad"])
        for d_ in range(2):
            for f_ in range(2):
                cp("dve", wpad[:, :, f_, 32 * d_:32 * d_ + 4], w[:, :, d_ * 8 + f_ * 4:d_ * 8 + f_ * 4 + 4], [wk, "wpad"], ["wpad"])
        for f_ in range(2):
            memset("dve", GW01[f_], 0.0, ["gw%d.0" % f_, "gw%d.1" % f_])
        for tc in range(4):
            for f_ in range(2):
                b = 6 + f_
                mmgroup([(psf(b)[0:36, :], wpad[:, c, f_, :], hT[:, c, tc * 512:(tc + 1) * 512], c == 0, c == 7) for c in range(8)],
                        hT_tc_keys(tc) + ["wpad"], ["ps%d" % b])
                act(GW01[f_][0:36, tc * 512:(tc + 1) * 512], psf(b)[0:36, :], AF.Identity, ["ps%d" % b, "gbc"],
                    ["gw%d.0" % f_, "gw%d.1" % f_], bias=gbc[0:36, f_:f_ + 1])
        dma("sp", hT_d, hT.rearrange("p c t -> p (c t)"), hT_keys, ["hT_d"])
        P.barrier()
        A.release("hT", "wbuf0", "wbuf1", "sq", "tmpq", "qn0", "qn1", "ssq", "sgo0", "sgo1", "xpb0", "xpb1", "dg", "gqb", "gkb", "wpad")

        if "p2" in debug:
            for nm, ap_, shp, dt_, keys in [
                ("qT", qT.rearrange("p a t -> p (a t)"), [128, 4 * S], BF16, []),
                ("kT", kT, [128, S], BF16, []),
                ("vaug", vaug.rearrange("p a b c -> p (a b c)"), [128, NT * 2 * 65], BF16, []),
                ("qmT", qmT.rearrange("p a t -> p (a t)"), [128, 4 * S], BF16, []),
                ("kmT", kmT.rearrange("p a t -> p (a t)"), [128, 4 * S], BF16, []),
                ("vaugm", vaugm.rearrange("p a b c -> p (a b c)"), [128, NT * 4 * 129], BF16, []),
            ]:
                d_ = dbg(nm, shp, dt_)
                dma("sp", d_, ap_, [], ["dbg_" + nm])
                outkeys.append("dbg_" + nm)
            d_ = dbg("sig", [S, 512], F32)
            dma("sp", d_, sig_d, ["sig_d.%d" % i for i in range(NT)], ["dbg_sig"])
            outkeys.append("dbg_sig")


        tabs = A.alloc("tabs", [8], F32)
        ohb = A.alloc("ohb", [512], F32)
        mrow = A.alloc("mrow", [512], F32)
        evec = A.alloc("evec", [512], F32)
        esink = A.alloc("esink", [8], F32)
        hk = A.alloc("hk", [8, 3, 128], F32)
        expb = A.alloc("expb", [3, 8, 128], F32)
        dma("sp", tabs[0:32, :], tab_d, [], ["tabs"])
        dma("sp", ohb[0:32, :], cst_d[0:32, C_OHB:C_OHB + 512], [], ["ohb"])
        dma("sp", mrow[0:8, :], cst_d[0:8, C_MROW:C_MROW + 512], [], ["mrow"])
        dma("sp", esink, sink_d.partition_broadcast(128), [], ["esink"])
        act(esink, esink, AF.Exp, ["esink"], ["esink"])
        mmgroup([(psf(0)[0:8, :], tabs[0:32, :], ohb[0:32, :], True, True)], ["tabs", "ohb"], ["ps0"])
        tt("dve", evec[0:8, :], psf(0)[0:8, :], mrow[0:8, :], ALU.add, ["ps0", "mrow"], ["evec"])
        act(evec[0:8, :], evec[0:8, :], AF.Exp, ["evec"], ["evec"])
        dma("sp", ev_d, evec[0:8, :], ["evec"], ["ev_d"])
        for h in range(8):
            dma("sp", hk[:, h, :, :], bass.AP(ev_d.tensor, h * 512, [[1, 128], [128, 3], [1, 128]]), ["ev_d"], ["hk.%d" % h])
        for j in range(3):
            for hh in range(2):
                b = 1 + ((j * 2 + hh) % 2)
                mmgroup([(psf(b)[:, a * 128:(a + 1) * 128], hk[:, hh * 4 + a, j, :], Jf, True, True) for a in range(4)],
                        ["hk.%d" % (hh * 4 + a) for a in range(4)] + ["Jf"], ["ps%d" % b])
                cp("dve", expb[:, j, hh * 4:hh * 4 + 4, :], psf(b).rearrange("p (a q) -> p a q", a=4), ["ps%d" % b], ["expb"])
        pck = 0
        for e_ in PRECAST:
            for (dst_, src_, c_, kn_) in ((wgs_d, weg_d, 8, "wgs"), (wus_d, weu_d, 8, "wus"), (wds_d, wed_d, 4, "wds")):
                o__ = P.op("pool", lambda e, o_=dst_[e_], i_=src_[e_].rearrange("(p c) n -> p (c n)", c=c_): e.dma_start(out=o_, in_=i_),
                           reads=["expb", "esink"] + (["pc.%d" % (pck - 2)] if pck >= 2 else []),
                           writes=["%s.%d" % (kn_, e_), "pc.%d" % pck], dma=True)
                o__.bg = True
                pck += 1
        yaT = A.alloc("yaT", [4, S], BF16)
        Es = [A.alloc("Es%d" % i, [512], F32) for i in range(2)]
        Pj = [A.alloc("Pj%d" % i, [512], BF16) for i in range(6)]
        yat = [A.alloc("yat%d" % i, [512], BF16) for i in range(2)]
        dens = A.alloc("dens", [8], F32)
        qkeys = ["qT.%d" % i for i in range(NT)]
        ecount = 0
        def at_sa(it):
            n, g = it // 2, it % 2
            js = [j for j in range(3) if 0 <= n - 1 + j < NT]
            for j in js:
                kb = n - 1 + j
                b = (it % 2) * 3 + j
                mmgroup([(psf(b), kT[64 * g:64 * g + 64, kb * 128:(kb + 1) * 128],
                          qT[64 * g:64 * g + 64, :, n * 128:(n + 1) * 128], True, True)],
                        ["kT.%d" % kb, "qT.%d" % n], ["ps%d" % b])
                ei = (it * 3 + j) % 2
                E_, ek = Es[ei], "Es%d" % ei
                act(E_, psf(b), AF.Exp, ["ps%d" % b], [ek])
                pj, pk = Pj[(it % 2) * 3 + j], "Pj%d" % ((it % 2) * 3 + j)
                tt("dve", pj.rearrange("p (a q) -> p a q", a=4), E_.rearrange("p (a q) -> p a q", a=4),
                   expb[:, j, 4 * g:4 * g + 4, :], ALU.mult, [ek, "expb"], [pk])

        def at_sb(it):
            n, g = it // 2, it % 2
            ya_t, yk = yat[n % 2], "yat%d" % (n % 2)
            js = [j for j in range(3) if 0 <= n - 1 + j < NT]
            ob = 6 + (it % 2)
            items = []
            for a in range(4):
                for jj, j in enumerate(js):
                    kb = n - 1 + j
                    items.append((psf(ob)[:, a * 65:(a + 1) * 65], Pj[(it % 2) * 3 + j][:, a * 128:(a + 1) * 128],
                                  vaug[:, kb, g, :], jj == 0, jj == len(js) - 1))
            mmgroup(items, ["Pj%d" % ((it % 2) * 3 + j) for j in js] + ["vaug.%d" % (n - 1 + j) for j in js] + ["vaug1"], ["ps%d" % ob])
            o3 = psf(ob)[:, 0:260].rearrange("p (a d) -> p a d", a=4)
            dk = "dens.%d" % g
            tt("dve", dens[:, 4 * g:4 * g + 4].unsqueeze(2), o3[:, :, 64:65], esink[:, 4 * g:4 * g + 4].unsqueeze(2), ALU.add,
               ["ps%d" % ob, "esink"], [dk])
            recip(dens[:, 4 * g:4 * g + 4], dens[:, 4 * g:4 * g + 4], [dk], [dk])
            tt("dve", ya_t[:, g * 256:(g + 1) * 256].rearrange("p (a d) -> p a d", a=4), o3[:, :, 0:64],
               dens[:, 4 * g:4 * g + 4].unsqueeze(2).broadcast_to([128, 4, 64]), ALU.mult, ["ps%d" % ob, dk], [yk + ".%d" % g])

        def at_sc(n):
            ya_t, yk = yat[n % 2], "yat%d" % (n % 2)
            tb = 6 + (n % 2)
            trgroup([(psb(tb)[:, c * 128:(c + 1) * 128], ya_t[:, c * 128:(c + 1) * 128], identb) for c in range(4)],
                    [yk + ".0", yk + ".1", "identb"], ["ps%d" % tb])
            cp("act", yaT[:, :, n * 128:(n + 1) * 128], psb(tb)[:, 0:512].rearrange("p (c t) -> p c t", c=4), ["ps%d" % tb], ["yaT.%d" % n])

        NIT = 2 * NT
        for t_ in range(NIT + 2):
            if t_ < NIT:
                at_sa(t_)
            if 0 <= t_ - 1 < NIT:
                at_sb(t_ - 1)
            if t_ >= 3 and (t_ - 1) % 2 == 0 and (t_ - 3) // 2 < NT:
                at_sc((t_ - 3) // 2)
        if "p3" in debug:
            d_ = dbg("yaT", [128, 4 * S], BF16)
            dma("sp", d_, yaT.rearrange("p c t -> p (c t)"), ["yaT.%d" % n for n in range(NT)], ["dbg_yaT"])
            outkeys.append("dbg_yaT")
            d_ = dbg("expb", [128, 3 * 8 * 128], F32)
            dma("sp", d_, expb.rearrange("p a b c -> p (a b c)"), ["expb"], ["dbg_expb"])
            outkeys.append("dbg_expb")
        P.barrier()
        A.release("tabs", "ohb", "mrow", "evec", "esink", "hk", "expb", "Es0", "Es1", "Pj0", "Pj1", "Pj2", "Pj3", "Pj4", "Pj5",
                  "yat0", "yat1", "dens", "qT", "kT", "vaug")


        GW = GW01 + [A.alloc("gw%d" % i, [S], F32) for i in range(2, 5)]
        ones1 = A.alloc("ones1", [1], F32)
        decs = A.alloc("decs", [16], F32)
        TS = A.alloc("TS", [3, NT, 36], F32)
        DECB = A.alloc("DECB", [8 * 16], F32)
        memset("dve", ones1, 1.0, ["ones1"])
        memset("dve", decs, 0.0, ["decs.0", "decs.1"])
        for i in range(2, 5):
            memset("dve", GW[i], 0.0, ["gw%d.0" % i, "gw%d.1" % i])
        def bk(kk):
            return ["gw%d.0" % kk, "gw%d.1" % kk]

        def both(kk):
            return GW[kk][0:36, :]
        act(both(1), both(1), AF.Exp, bk(1), bk(1), scale=-1.0)
        act(both(1), both(1), AF.Ln, bk(1), bk(1), bias=1.0)
        for d in range(2):
            p0 = 32 * d
            o_, i_ = GW[2][p0:p0 + 4, :], GW[1][p0:p0 + 4, :]
            if d == 1:
                o_, i_ = o_[:, ::-1], i_[:, ::-1]
            onesbc = ones1[p0:p0 + 4, 0:1].broadcast_to([4, S])
            P.op("dve", lambda e, o_=o_, i_=i_, ob=onesbc: e.tensor_tensor_scan(o_, ob, i_, 0.0, ALU.mult, ALU.subtract),
                 reads=["gw1.%d" % d, "ones1"], writes=["gw2.%d" % d])
        tt("dve", both(1), both(0), both(2), ALU.subtract, bk(0) + bk(2), bk(1))
        for d in range(2):
            p0 = 32 * d
            o_, i_ = GW[0][p0:p0 + 4, :], GW[1][p0:p0 + 4, :]
            if d == 1:
                o_, i_ = o_[:, ::-1], i_[:, ::-1]
            onesbc = ones1[p0:p0 + 4, 0:1].broadcast_to([4, S])
            P.op("dve", lambda e, o_=o_, i_=i_, ob=onesbc: e.tensor_tensor_scan(o_, ob, i_, 0.0, ALU.mult, ALU.max),
                 reads=["gw1.%d" % d, "ones1"], writes=["gw0.%d" % d])
            A3 = GW[0][p0:p0 + 4, :].rearrange("p (c t) -> p c t", c=NT)
            Mp3 = GW[3][p0:p0 + 4, :].rearrange("p (c t) -> p c t", c=NT)
            Mn3 = GW[4][p0:p0 + 4, :].rearrange("p (c t) -> p c t", c=NT)
            if d == 0:
                cp("dve", Mp3[:, 1:NT, :], A3[:, 0:NT - 1, 127:128].broadcast_to([4, NT - 1, 128]), ["gw0.0"], ["gw3.0"])
                cp("dve", Mn3, A3[:, :, 127:128].broadcast_to([4, NT, 128]), ["gw0.0"], ["gw4.0"])
            else:
                cp("dve", Mp3[:, 0:NT - 1, :], A3[:, 1:NT, 0:1].broadcast_to([4, NT - 1, 128]), ["gw0.1"], ["gw3.1"])
                cp("dve", Mn3, A3[:, :, 0:1].broadcast_to([4, NT, 128]), ["gw0.1"], ["gw4.1"])
        Mpb = both(3).rearrange("p (c t) -> p c t", c=NT)
        Mnb = both(4).rearrange("p (c t) -> p c t", c=NT)
        tt("dve", decs[0:36, :].unsqueeze(2), Mpb[:, :, 0:1], Mnb[:, :, 0:1], ALU.subtract, bk(3) + bk(4), ["decs.0", "decs.1"])
        act(decs[0:36, :], decs[0:36, :], AF.Exp, ["decs.0", "decs.1"], ["decs.0", "decs.1"])
        tt("dve", both(0), both(1), both(3), ALU.subtract, bk(1) + bk(3), bk(0))
        act(both(0), both(0), AF.Exp, bk(0), bk(0), bias=LNS)
        tt("dve", both(4), both(1), both(4), ALU.subtract, bk(1) + bk(4), bk(4))
        act(both(4), both(4), AF.Exp, bk(4), bk(4), bias=LNS)
        tt("dve", both(2), both(2), both(3), ALU.add, bk(2) + bk(3), bk(2))
        act(both(2), both(2), AF.Exp, bk(2), bk(2), scale=-1.0)
        if "p4pre" in debug:
            for i_ in range(5):
                d_ = dbg("gw%d" % i_, [128, S], F32)
                dma("sp", d_, GW[i_], ["gw%d.0" % i_, "gw%d.1" % i_], ["dbg_gw%d" % i_])
                outkeys.append("dbg_gw%d" % i_)
        checkpoint("p4pre")
        Esel = A.alloc("Esel", [8, 128], F32)
        for li, l_ in enumerate((0, 1, 2, 3, 32, 33, 34, 35)):
            cp("dve", Esel[0:36, li, :], identf[0:36, l_:l_ + 1].broadcast_to([36, 128]), ["identf"], ["Esel"])
        mmgroup([(psf(7)[:, li * 16:(li + 1) * 16], Esel[0:36, li, :], decs[0:36, :], True, True) for li in range(8)],
                ["Esel", "decs.0", "decs.1"], ["ps7"])
        cp("dve", DECB, psf(7)[:, 0:128], ["ps7"], ["DECB"])
        for ki, slot in enumerate((0, 4, 2)):
            for half in range(2):
                b = (ki * 2 + half) % 4
                trgroup([(psf(b)[:, ii * 36:(ii + 1) * 36], GW[slot][0:36, (half * 8 + ii) * 128:(half * 8 + ii + 1) * 128], identf[0:36, 0:36])
                         for ii in range(8)], ["gw%d.0" % slot, "gw%d.1" % slot, "identf"], ["ps%d" % b])
                cp("dve", TS[:, ki, half * 8:(half + 1) * 8, :], psf(b)[:, 0:288].rearrange("p (i c) -> p i c", i=8), ["ps%d" % b], ["TS"])
        if "p4a" in debug:
            d_ = dbg("TS", [128, 3 * NT * 36], F32)
            dma("sp", d_, TS.rearrange("p a b c -> p (a b c)"), ["TS"], ["dbg_TS"])
            outkeys.append("dbg_TS")
            d_ = dbg("DECB", [128, 8 * 16], F32)
            dma("sp", d_, DECB, ["DECB"], ["dbg_DECB"])
            outkeys.append("dbg_DECB")
        checkpoint("p4a")
        P.barrier()
        A.release("gw0", "gw1", "gw2", "gw3", "gw4", "ones1", "decs", "Esel")

        ktok = A.alloc("ktok", [NT, 4, 128], BF16)
        vu = A.alloc("vu", [2, NT, 4, 129], BF16)
        vu2 = A.alloc("vu2", [2, NT, 4, 129], BF16)
        for i in range(NT):
            b = i % 2
            trgroup([(psb(b)[:, h * 128:(h + 1) * 128], kmT[:, h, i * 128:(i + 1) * 128], identb) for h in range(4)],
                    ["kmT.%d" % h for h in range(4)] + ["identb"], ["ps%d" % b])
            cp("act", ktok[:, i, :, :], psb(b)[:, 0:512].rearrange("p (h d) -> p h d", h=4), ["ps%d" % b], ["ktok.%d" % i])
            for d in range(2):
                tt("dve", vu[:, d, i, :, :], vaugm[:, i, :, :], TS[:, 0, i, d * 32:d * 32 + 4].unsqueeze(2).broadcast_to([128, 4, 129]),
                   ALU.mult, ["vaugm.%d" % i, "vaugm1", "TS"], ["vu.%d.%d" % (d, i)])
                tt("dve", vu2[:, d, i, :, :], vaugm[:, i, :, :], TS[:, 1, i, d * 32:d * 32 + 4].unsqueeze(2).broadcast_to([128, 4, 129]),
                   ALU.mult, ["vaugm.%d" % i, "vaugm1", "TS"], ["vu2.%d.%d" % (d, i)])
        P.barrier()
        A.release("vaugm")
        Hparts = [A.alloc("Hh%d" % i, [NT // 2, 512], F32) for i in range(2)]

        def Hat(c_):
            return Hparts[c_ // (NT // 2)][:, c_ % (NT // 2), :]
        C32 = A.alloc("C32", [2, 4, 128], F32)
        n32 = A.alloc("n32", [2, 4], F32)
        Cbf = A.alloc("Cbf", [2, 4, 128], BF16)
        nbf = A.alloc("nbf", [2, 4], BF16)
        Smb = [A.alloc("Sm%d" % i, [4, 128], BF16) for i in range(2)]
        tmpH = [A.alloc("tmpH%d" % i, [4, 128], F32) for i in range(2)]
        tmp1 = A.alloc("tmp1", [2, 2, 4], F32)
        DECB3 = DECB.rearrange("p (l c) -> p l c", c=16)
        checkpoint("p4b")
        for step in range(NT):
            for d in range(2):
                c = step if d == 0 else NT - 1 - step
                cs = slice(c * 128, (c + 1) * 128)
                SA, OB, DB, CB = 4 * d, 4 * d + 1, 4 * d + 2, 4 * d + 3
                kSA, kOB, kDB, kCB = "ps%d" % SA, "ps%d" % OB, "ps%d" % DB, "ps%d" % CB
                qk_ = ["qmT.%d" % h for h in range(4)]
                mmgroup([(psf(SA)[:, h * 128:(h + 1) * 128], kmT[:, h, cs], qmT[:, h, cs], True, True) for h in range(4)],
                        ["kmT.%d" % h for h in range(4)] + qk_, [kSA])
                sm, smk = Smb[d], "Sm%d" % d
                msk = (maskf if d == 0 else maskb).unsqueeze(1).broadcast_to([128, 4, 128])
                tt("dve", sm, psf(SA).rearrange("p (h t) -> p h t", h=4), msk, ALU.mult, [kSA, "maskf", "maskb"], [smk])
                items, itemsd = [], []
                for h in range(4):
                    items.append((psf(OB)[:, h * 128:(h + 1) * 128], sm[:, h, :], vu[:, d, c, h, 0:128], True, step == 0))
                    if step > 0:
                        items.append((psf(OB)[:, h * 128:(h + 1) * 128], qmT[:, h, cs], Cbf[:, d, h, :], False, True))
                for h in range(4):
                    itemsd.append((psf(DB)[:, h:h + 1], sm[:, h, :], vu[:, d, c, h, 128:129], True, step == 0))
                    if step > 0:
                        itemsd.append((psf(DB)[:, h:h + 1], qmT[:, h, cs], nbf[:, d, h:h + 1], False, True))
                mmgroup(items, [smk, "vu.%d.%d" % (d, c)] + (qk_ + ["Cbf.%d" % d] if step > 0 else []), [kOB])
                mmgroup(itemsd, [smk, "vu.%d.%d" % (d, c)] + (qk_ + ["nbf.%d" % d] if step > 0 else []), [kDB])
                t1, rc = tmp1[:, d, 0, :], tmp1[:, d, 1, :]
                tk = "tmp1.%d" % d
                tt("dve", t1, psf(DB)[:, 0:4], TS[:, 2, c, d * 32:d * 32 + 4], ALU.max, [kDB, "TS"], [tk])
                stt(t1, psf(DB)[:, 0:4], -1.0, t1, ALU.mult, ALU.max, [kDB, tk], [tk])
                recip(rc, t1, [tk], [tk + "r"])
                hk_ = "H.%d" % c
                hdst = Hat(c).rearrange("p (h e) -> p h e", h=4)
                rcb = rc.unsqueeze(2).broadcast_to([128, 4, 128])
                first = (d == 0 and c < NT // 2) or (d == 1 and c >= NT // 2)
                if first:
                    tt("dve", hdst, psf(OB).rearrange("p (h e) -> p h e", h=4), rcb, ALU.mult, [kOB, tk + "r"], [hk_])
                else:
                    th, thk = tmpH[d], "tmpH%d" % d
                    tt("dve", th, psf(OB).rearrange("p (h e) -> p h e", h=4), rcb, ALU.mult, [kOB, tk + "r"], [thk])
                    tt("dve", hdst, hdst, th, ALU.add, [hk_, thk], [hk_])
                if step < NT - 1:
                    mmgroup([(psf(CB)[:, h * 128:(h + 1) * 128], ktok[:, c, h, :], vu2[:, d, c, h, 0:128], True, True) for h in range(4)],
                            ["ktok.%d" % c, "vu2.%d.%d" % (d, c)], [kCB])
                    mmgroup([(psf(DB)[:, 4 + h:5 + h], ktok[:, c, h, :], vu2[:, d, c, h, 128:129], True, True) for h in range(4)],
                            ["ktok.%d" % c, "vu2.%d.%d" % (d, c)], [kDB])
                    ck, nk = "C32.%d" % d, "n32.%d" % d
                    c32 = C32[:, d, :, :]
                    if step == 0:
                        cp("dve", c32, psf(CB).rearrange("p (h e) -> p h e", h=4), [kCB], [ck])
                        cp("dve", n32[:, d, :], psf(DB)[:, 4:8], [kDB], [nk])
                    else:
                        dec4 = DECB3[:, d * 4:d * 4 + 4, c]
                        tt("dve", c32, c32, dec4.unsqueeze(2).broadcast_to([128, 4, 128]), ALU.mult, [ck, "DECB"], [ck])
                        tt("dve", c32, c32, psf(CB).rearrange("p (h e) -> p h e", h=4), ALU.add, [ck, kCB], [ck])
                        tt("dve", n32[:, d, :], n32[:, d, :], dec4, ALU.mult, [nk, "DECB"], [nk])
                        tt("dve", n32[:, d, :], n32[:, d, :], psf(DB)[:, 4:8], ALU.add, [nk, kDB], [nk])
                    cp("act", Cbf[:, d, :, :], c32, [ck], ["Cbf.%d" % d])
                    cp("act", nbf[:, d, :], n32[:, d, :], [nk], ["nbf.%d" % d])
        if "p4" in debug:
            d_ = dbg("H", [128, NT * 512], F32)
            for hp_ in range(2):
                dma("sp", d_[:, hp_ * 4096:(hp_ + 1) * 4096], Hparts[hp_].rearrange("p a b -> p (a b)"), ["H.%d" % c for c in range(NT)], ["dbg_H%d" % hp_])
                outkeys.append("dbg_H%d" % hp_)
        checkpoint("p4c")
        P.barrier()
        A.release("ktok", "vu", "vu2", "C32", "n32", "Cbf", "nbf", "Sm0", "Sm1", "tmpH0", "tmpH1", "tmp1", "qmT", "kmT", "TS", "DECB")
        ymT = A.alloc("ymT", [4, S], BF16)
        ogb = A.alloc("ogb", [512], F32)
        sqh = A.alloc("sqh", [512], F32)
        tmph = A.alloc("tmph", [512], F32)
        sgt_all = A.alloc("sgtall", [NT, 512], F32)
        sgt = [sgt_all[:, i, :] for i in range(NT)]
        ymt = [A.alloc("ymt%d" % i, [512], BF16) for i in range(2)]
        ss4 = A.alloc("ss4", [4], F32)
        dma("sp", ogb, og_d.partition_broadcast(128), [], ["ogb"])
        for i in range(NT):
            dma("sp", sgt[i], sig_d[i * 128:(i + 1) * 128, :], ["sig_d.%d" % i], ["sgt%d" % i])
        for i in range(NT):
            hkeys = ["H.%d" % i]
            act(sqh, Hat(i), AF.Square, hkeys, ["sqh"])
            P.op("dve", lambda e: e.tensor_reduce(ss4, sqh.rearrange("p (h d) -> p h d", h=4), AX.X, ALU.add), reads=["sqh"], writes=["ss4"])
            act(ss4, ss4, AF.Sqrt, ["ss4"], ["ss4"], bias=EPS, scale=1.0 / 128)
            recip(ss4, ss4, ["ss4"], ["ss4"])
            tt("dve", tmph.rearrange("p (h d) -> p h d", h=4), Hat(i).rearrange("p (h d) -> p h d", h=4),
               ss4.unsqueeze(2).broadcast_to([128, 4, 128]), ALU.mult, hkeys + ["ss4"], ["tmph"])
            tt("dve", tmph, tmph, ogb, ALU.mult, ["tmph", "ogb"], ["tmph"])
            tt("dve", ymt[i % 2], tmph, sgt[i], ALU.mult, ["tmph", "sgt%d" % i], ["ymt%d" % (i % 2)])
            b = i % 2
            trgroup([(psb(b)[:, c * 128:(c + 1) * 128], ymt[i % 2][:, c * 128:(c + 1) * 128], identb) for c in range(4)],
                    ["ymt%d" % (i % 2), "identb"], ["ps%d" % b])
            cp("act", ymT[:, :, i * 128:(i + 1) * 128], psb(b)[:, 0:512].rearrange("p (c t) -> p c t", c=4), ["ps%d" % b], ["ymT.%d" % i])
        if "p4" in debug:
            d_ = dbg("ymT", [128, 4 * S], BF16)
            dma("sp", d_, ymT.rearrange("p c t -> p (c t)"), ["ymT.%d" % n for n in range(NT)], ["dbg_ymT"])
            outkeys.append("dbg_ymT")
        P.barrier()
        A.release("Hh0", "Hh1", "ogb", "sqh", "tmph", "sgtall", "ymt0", "ymt1", "ss4")


        hT2 = A.alloc("hT2", [8, S], BF16)
        yaT2 = yaT
        Wa = A.alloc("Wa", [4, D], BF16)
        Wm = A.alloc("Wm", [4, D], BF16)
        Wout = A.alloc("Wout", [8, D], BF16)
        wmg = [A.alloc("wmg%d" % i, [8, 2, 128], BF16) for i in range(2)]
        uT = A.alloc("uT", [8, S], BF16)
        gaS = [A.alloc("gaS%d" % i, [512], F32) for i in range(2)]
        gmS = [A.alloc("gmS%d" % i, [512], F32) for i in range(2)]
        u1S = [A.alloc("u1S%d" % i, [512], F32) for i in range(2)]
        u2S = [A.alloc("u2S%d" % i, [512], F32) for i in range(2)]
        dma("sp", hT2.rearrange("p c t -> p (c t)"), hT_d, ["hT_d"], ["hT2"])
        dma("pool", Wa, wview(wa_d), [], ["Wa"])
        dma("pool", Wm, wview(wm_d), [], ["Wm"])
        dma("pool", Wout, wview(wo_d), [], ["Wout"])
        ymkeys = ["ymT.%d" % n for n in range(NT)]
        mgsrc = wview(win_d)[:, :, 2832:4880].rearrange("p c (two f) -> p c two f", two=2)
        for fc in range(8):
            wg_, wgk = wmg[fc % 2], "wmg%d" % (fc % 2)
            for two in range(2):
                c0 = 2832 + two * 1024 + fc * 128
                dma("pool", wg_[:, :, two, :], wview(win_d)[:, :, c0:c0 + 128], [], [wgk + ".%d" % two])
            fs = slice(fc * 128, (fc + 1) * 128)
            for tc in range(4):
                it = fc * 4 + tc
                base = (it % 2) * 4
                tcs = slice(tc * 512, (tc + 1) * 512)
                mmgroup([(psf(base), Wa[:, kc, fs], yaT2[:, kc, tcs], kc == 0, kc == 3) for kc in range(4)], ["Wa"] + ["yaT.%d" % n_ for n_ in range(tc * 4, tc * 4 + 4)], ["ps%d" % base])
                mmgroup([(psf(base + 1), Wm[:, kc, fs], ymT[:, kc, tcs], kc == 0, kc == 3) for kc in range(4)],
                        ["Wm"] + ymkeys[tc * 4:tc * 4 + 4], ["ps%d" % (base + 1)])
                mmgroup([(psf(base + 2), wg_[:, c, 0, :], hT2[:, c, tcs], c == 0, c == 7) for c in range(8)], [wgk + ".0", "hT2"], ["ps%d" % (base + 2)])
                mmgroup([(psf(base + 3), wg_[:, c, 1, :], hT2[:, c, tcs], c == 0, c == 7) for c in range(8)], [wgk + ".1", "hT2"], ["ps%d" % (base + 3)])
                x2 = it % 2
                act(gaS[x2], psf(base + 2), AF.Sigmoid, ["ps%d" % (base + 2), "prm"], ["gaS%d" % x2], bias=prm[:, 48 + fc:49 + fc])
                act(gmS[x2], psf(base + 3), AF.Sigmoid, ["ps%d" % (base + 3), "prm"], ["gmS%d" % x2], bias=prm[:, 56 + fc:57 + fc])
                tt("dve", u1S[x2], gaS[x2], psf(base), ALU.mult, ["gaS%d" % x2, "ps%d" % base], ["u1S%d" % x2])
                tt("dve", u2S[x2], gmS[x2], psf(base + 1), ALU.mult, ["gmS%d" % x2, "ps%d" % (base + 1)], ["u2S%d" % x2])
                tt("dve", uT[:, fc, tcs], u1S[x2], u2S[x2], ALU.add, ["u1S%d" % x2, "u2S%d" % x2], ["uT.%d.%d" % (fc, tc)])
        if "p5" in debug:
            d_ = dbg("uT", [128, 8 * S], BF16)
            dma("sp", d_, uT.rearrange("p c t -> p (c t)"), ["uT.%d.%d" % (fc, tc) for fc in range(8) for tc in range(4)], ["dbg_uT"])
            outkeys.append("dbg_uT")
        checkpoint("p5")

        P.barrier()
        A.release("hT2", "yaT", "Wa", "Wm", "wmg0", "wmg1", "gaS0", "gaS1", "gmS0", "gmS1", "u1S0", "u1S1", "u2S0", "u2S1", "ymT")
        xts2 = [A.alloc("xts2_%d" % i, [D], F32) for i in range(2)]
        x1t = [A.alloc("x1t%d" % i, [D], F32) for i in range(2)]
        xnf = [A.alloc("xnf%d" % i, [D], F32) for i in range(2)]
        xnball = A.alloc("xnball", [NT, D], BF16)
        xnT = [A.alloc("xnT%d" % i, [8, 128], F32) for i in range(2)]
        junk2 = A.alloc("junk2", [D], BF16)
        Wr = A.alloc("Wr", [8, 36], F32)
        rbb = A.alloc("rbb", [36], F32)
        DST = A.alloc("DST", [NT, 2], I32)
        WTS = A.alloc("WTS", [NT, 2], F32)
        ss2 = A.alloc("ss2", [NT], F32)
        Lall = A.alloc("Lall", [NT, 36], F32)
        dma("sp", Wr[:, :, 0:4], wrg_d.rearrange("(c p) n -> p c n", p=128), [], ["Wr_a"])
        dma("sp", Wr[:, :, 4:36], wre_d.rearrange("(c p) n -> p c n", p=128), [], ["Wr_b"])
        dma("sp", rbb[:, 0:4], brg_d.partition_broadcast(128), [], ["rbb_a"])
        dma("sp", rbb[:, 4:36], bre_d.partition_broadcast(128), [], ["rbb_b"])
        uTk = lambda i: ["uT.%d.%d" % (fc, i // 4) for fc in range(8)]

        def r_s1(i):
            xt, xk = xts2[i % 2], "xts2_%d" % (i % 2)
            x1, x1k = x1t[i % 2], "x1t%d" % (i % 2)
            dma("sp", xt, x_d[i * 128:(i + 1) * 128, :], [], [xk])
            for half in range(2):
                b = 2 * (i % 2) + half
                hs = slice(half * 512, (half + 1) * 512)
                mmgroup([(psf(b), uT[:, c, i * 128:(i + 1) * 128], Wout[:, c, hs], c == 0, c == 7) for c in range(8)],
                        uTk(i) + ["Wout"], ["ps%d" % b])
                tt("dve", x1[:, hs], psf(b), xt[:, hs], ALU.add, ["ps%d" % b, xk], [x1k + ".%d" % half])
            x1ks = [x1k + ".0", x1k + ".1"]
            dma("sp", x1_d[i * 128:(i + 1) * 128, :], x1, x1ks, ["x1_d.%d" % i])
            act(junk2, x1, AF.Square, x1ks, ["junk2", "ss2.%d" % i], accum_out=ss2[:, i:i + 1])
            act(ss2[:, i:i + 1], ss2[:, i:i + 1], AF.Sqrt, ["ss2.%d" % i], ["ss2.%d" % i], bias=EPS, scale=1.0 / D)
            recip(ss2[:, i:i + 1], ss2[:, i:i + 1], ["ss2.%d" % i], ["ss2.%d" % i])
            stt(xnf[i % 2], x1, ss2[:, i:i + 1], g2b, ALU.mult, ALU.mult, x1ks + ["ss2.%d" % i, "g2b"], ["xnf%d" % (i % 2)])
            cp("act", xnball[:, i, :], xnf[i % 2], ["xnf%d" % (i % 2)], ["xnb.%d" % i])

        def r_s2(i):
            xf, xfk = xnf[i % 2], "xnf%d" % (i % 2)
            trgroup([(psf(4)[:, c * 128:(c + 1) * 128], xf[:, c * 128:(c + 1) * 128], identf) for c in range(4)], [xfk, "identf"], ["ps4"])
            trgroup([(psf(5)[:, c * 128:(c + 1) * 128], xf[:, (4 + c) * 128:(5 + c) * 128], identf) for c in range(4)], [xfk, "identf"], ["ps5"])
            cp("dve", xnT[i % 2][:, 0:4, :], psf(4).rearrange("p (c t) -> p c t", c=4), ["ps4"], ["xnT%d.a" % (i % 2)])
            cp("act", xnT[i % 2][:, 4:8, :], psf(5).rearrange("p (c t) -> p c t", c=4), ["ps5"], ["xnT%d.b" % (i % 2)])

        def r_s3(i):
            mmgroup([(psf(6)[:, 0:36], xnT[i % 2][:, c, :], Wr[:, c, :], c == 0, c == 7) for c in range(8)],
                    ["xnT%d.a" % (i % 2), "xnT%d.b" % (i % 2), "Wr_a", "Wr_b"], ["ps6"])
            tt("dve", Lall[:, i, :], psf(6)[:, 0:36], rbb, ALU.add, ["ps6", "rbb_a", "rbb_b"], ["Lall.%d" % i])

        for t_ in range(NT + 2):
            for off, stg in ((0, r_s1), (1, r_s2), (2, r_s3)):
                if 0 <= t_ - off < NT:
                    stg(t_ - off)
        Lk = ["Lall.%d" % i for i in range(NT)]
        Rs = A.alloc("Rs", [12, NT], F32)
        G4 = A.alloc("G4", [3, NT, 4], F32)
        B32 = A.alloc("B32", [8, NT, 32], F32)
        OHb = A.alloc("OHb", [NT, 32], BF16)
        gmax, sgs_, pg, v1, v2, e2, w1, w2 = (Rs[:, j, :] for j in range(8))
        d1f, d2f, ok1, ok2 = (Rs[:, j, :] for j in range(8, 12))
        ohg, pen, eg = G4[:, 0], G4[:, 1], G4[:, 2]
        em, em2, oh1, oh2, OHs, rin, dfull, tmp32 = (B32[:, j] for j in range(8))
        Lg = Lall[:, :, 0:4]
        Le = Lall[:, :, 4:36]

        def bc3(ap, n):
            return ap.unsqueeze(2).broadcast_to([128, NT, n])

        def red(out, in_, op, reads, writes):
            P.op("dve", lambda e: e.tensor_reduce(out, in_, AX.X, op), reads=reads, writes=writes)
        red(gmax, Lg, ALU.max, Lk, ["q.gmax"])
        tt("dve", ohg, Lg, bc3(gmax, 4), ALU.is_equal, Lk + ["q.gmax"], ["q.ohg"])
        tt("dve", eg, Lg, bc3(gmax, 4), ALU.subtract, Lk + ["q.gmax"], ["q.eg"])
        act(eg, eg, AF.Exp, ["q.eg"], ["q.eg"])
        red(sgs_, eg, ALU.add, ["q.eg"], ["q.sg"])
        recip(pg, sgs_, ["q.sg"], ["q.pg"])
        ts("dve", pen, ohg, -1.0, 1e30, ALU.add, ALU.mult, ["q.ohg"], ["q.pen"])
        tt("dve", em.rearrange("p i (g j) -> p i g j", g=4), Le.rearrange("p i (g j) -> p i g j", g=4),
           pen.unsqueeze(3).broadcast_to([128, NT, 4, 8]), ALU.add, Lk + ["q.pen"], ["q.em"])
        red(v1, em, ALU.max, ["q.em"], ["q.v1"])
        tt("dve", oh1, em, bc3(v1, 32), ALU.is_equal, ["q.em", "q.v1"], ["q.oh1"])
        stt(em2, oh1, -1e30, em, ALU.mult, ALU.add, ["q.oh1", "q.em"], ["q.em2"])
        red(v2, em2, ALU.max, ["q.em2"], ["q.v2"])
        tt("dve", oh2, em2, bc3(v2, 32), ALU.is_equal, ["q.em2", "q.v2"], ["q.oh2"])
        tt("dve", e2, v2, v1, ALU.subtract, ["q.v1", "q.v2"], ["q.e2"])
        act(e2, e2, AF.Exp, ["q.e2"], ["q.e2"])
        ts("dve", w1, e2, 1.0, None, ALU.add, None, ["q.e2"], ["q.w1"])
        recip(w1, w1, ["q.w1"], ["q.w1"])
        tt("dve", w1, w1, pg, ALU.mult, ["q.w1", "q.pg"], ["q.w1"])
        tt("dve", w2, w1, e2, ALU.mult, ["q.w1", "q.e2"], ["q.w2"])
        tt("dve", OHs, oh1, oh2, ALU.add, ["q.oh1", "q.oh2"], ["q.OH"])
        cp("dve", OHb, OHs, ["q.OH"], ["q.OHb"])
        items = []
        for i in range(NT):
            sl_ = psf(7)[:, i * 32:(i + 1) * 32]
            items.append((sl_, trib, OHb[:, i, :], True, i == 0))
            for j in range(i):
                items.append((sl_, onesb, OHb[:, j, :], False, j == i - 1))
        mmgroup(items, ["trib", "onesb", "q.OHb"], ["ps7"])
        cp("dve", rin, psf(7).rearrange("p (i e) -> p i e", i=NT), ["ps7"], ["q.rin"])
        tt("dve", dfull, rin, basee.unsqueeze(1).broadcast_to([128, NT, 32]), ALU.add, ["q.rin", "basee"], ["q.dfull"])
        ts("dve", rin, rin, float(CAPR), None, ALU.is_lt, None, ["q.rin"], ["q.okm"])
        ts("dve", dfull, dfull, -float(TRASH), None, ALU.add, None, ["q.dfull"], ["q.dfull"])
        tt("dve", dfull, dfull, rin, ALU.mult, ["q.dfull", "q.okm"], ["q.dfull"])
        ts("dve", dfull, dfull, float(TRASH), None, ALU.add, None, ["q.dfull"], ["q.dfull"])
        for (oh_, src_, dst_, kn) in ((oh1, dfull, d1f, "d1f"), (oh2, dfull, d2f, "d2f"), (oh1, rin, ok1, "ok1"), (oh2, rin, ok2, "ok2")):
            tt("dve", tmp32, oh_, src_, ALU.mult, ["q.oh1", "q.oh2", "q.dfull", "q.okm"], ["q.tmp32"])
            red(dst_, tmp32, ALU.add, ["q.tmp32"], ["q." + kn])
        tt("dve", WTS[:, :, 0], w1, ok1, ALU.mult, ["q.w1", "q.ok1"], ["WTS.a"])
        tt("dve", WTS[:, :, 1], w2, ok2, ALU.mult, ["q.w2", "q.ok2"], ["WTS.b"])
        cp("dve", DST[:, :, 0], d1f, ["q.d1f"], ["DST.a"])
        cp("dve", DST[:, :, 1], d2f, ["q.d2f"], ["DST.b"])
        for i in range(NT):
            for kk in range(2):
                P.op("pool", lambda e, o_=DST[:, i, kk:kk + 1], x_=xnball[:, i, :]: e.indirect_dma_start(
                    out=xs_d, out_offset=bass.IndirectOffsetOnAxis(ap=o_, axis=0), in_=x_, in_offset=None),
                    reads=["xnb.%d" % i, "DST.a", "DST.b", "xs"], writes=["xs.%d.%d" % (i, kk)], dma=True)
        xskeys = ["xs.%d.%d" % (i, kk) for i in range(NT) for kk in range(2)]
        if "p6a" in debug:
            d_ = dbg("DST", [128, NT * 2], I32)
            dma("sp", d_, DST.rearrange("p a b -> p (a b)"), ["DST.a", "DST.b"], ["dbg_DST"])
            outkeys.append("dbg_DST")
            d_ = dbg("WTS", [128, NT * 2], F32)
            dma("sp", d_, WTS.rearrange("p a b -> p (a b)"), ["WTS.a", "WTS.b"], ["dbg_WTS"])
            outkeys.append("dbg_WTS")
            d_ = dbg("x1", [S, D], F32)
            dma("sp", d_, x1_d, ["x1_d.%d" % i for i in range(NT)], ["dbg_x1"])
            outkeys.append("dbg_x1")
        checkpoint("p6a")
        P.barrier()
        A.release("Wout", "uT", "xts2_0", "xts2_1", "x1t0", "x1t1", "xnf0", "xnf1", "xnball", "xnT0", "xnT1", "junk2", "Wr", "rbb", "ss2",
                  "Lall", "Rs", "G4", "B32", "OHb")

        NWB = 4
        wgb = [A.alloc("wgb%d" % i, [8, 512], BF16) for i in range(NWB)]
        wub = [A.alloc("wub%d" % i, [8, 512], BF16) for i in range(NWB)]
        wdb = [A.alloc("wdb%d" % i, [4, D], BF16) for i in range(NWB)]
        xbr = [A.alloc("xbr%d" % i, [2, D], BF16) for i in range(3)]
        xbT = [A.alloc("xbT%d" % i, [8, CAPR], BF16) for i in range(3)]
        actT = [A.alloc("actT%d" % i, [4, CAPR], BF16) for i in range(2)]
        sgs = [A.alloc("sgs%d" % i, [CAPR], F32) for i in range(2)]
        ysb = [A.alloc("ysb%d" % i, [2, D], F32) for i in range(2)]
        NXB = 3

        def stageL(e_):
            s3, x3 = e_ % NWB, e_ % NXB
            dma("sp", xbr[x3], xs_d[e_ * CAPR:(e_ + 1) * CAPR, :].rearrange("(r p) d -> p r d", p=128), xskeys + ["xs"], ["xbr%d" % x3])
            if e_ in PRECAST:
                dma("sp", wgb[s3], wgs_d[e_].rearrange("p (c n) -> p c n", c=8), ["wgs.%d" % e_], ["wgb%d" % s3])
                dma("sp", wub[s3], wus_d[e_].rearrange("p (c n) -> p c n", c=8), ["wus.%d" % e_], ["wub%d" % s3])
                dma("sp", wdb[s3], wds_d[e_].rearrange("p (c n) -> p c n", c=4), ["wds.%d" % e_], ["wdb%d" % s3])
            else:
                dma("pool", wgb[s3], weg_d[e_].rearrange("(p c) n -> p c n", c=8), [], ["wgb%d" % s3])
                dma("pool", wub[s3], weu_d[e_].rearrange("(p c) n -> p c n", c=8), [], ["wub%d" % s3])
                dma("pool", wdb[s3], wed_d[e_].rearrange("(p c) n -> p c n", c=4), [], ["wdb%d" % s3])

        def stageT(e_):
            x3 = e_ % NXB
            for r in range(2):
                trgroup([(psb(r)[:, c * 128:(c + 1) * 128], xbr[x3][:, r, c::8], identb) for c in range(8)],
                        ["xbr%d" % x3, "identb"], ["ps%d" % r])
                cp("act" if r == 0 else "dve", xbT[x3][:, :, r * 128:(r + 1) * 128], psb(r).rearrange("p (c t) -> p c t", c=8),
                   ["ps%d" % r], ["xbT%d.%d" % (x3, r)])

        def stageB(e_):
            s3, x2, x3 = e_ % NWB, e_ % 2, e_ % NXB
            xtk = ["xbT%d.0" % x3, "xbT%d.1" % x3]
            for f in range(4):
                bg, bu = 2 + (f % 2) * 2, 3 + (f % 2) * 2
                fs = slice(f, 512, 4)
                mmgroup([(psf(bg)[:, 0:CAPR], wgb[s3][:, c, fs], xbT[x3][:, c, :], c == 0, c == 7) for c in range(8)], ["wgb%d" % s3] + xtk, ["ps%d" % bg])
                mmgroup([(psf(bu)[:, 0:CAPR], wub[s3][:, c, fs], xbT[x3][:, c, :], c == 0, c == 7) for c in range(8)], ["wub%d" % s3] + xtk, ["ps%d" % bu])
                act(sgs[f % 2], psf(bg)[:, 0:CAPR], AF.Silu, ["ps%d" % bg], ["sgs%d" % (f % 2)])
                tt("dve", actT[x2][:, f, :], sgs[f % 2], psf(bu)[:, 0:CAPR], ALU.mult, ["sgs%d" % (f % 2), "ps%d" % bu], ["actT%d.%d" % (x2, f)])

        def stageC(e_):
            s3, x2 = e_ % NWB, e_ % 2
            atk = ["actT%d.%d" % (x2, f) for f in range(4)]
            for r in range(2):
                for half in range(2):
                    b = 6 + (r * 2 + half) % 2
                    hs = slice(half * 512, (half + 1) * 512)
                    mmgroup([(psf(b), actT[x2][:, f, r * 128:(r + 1) * 128], wdb[s3][:, f, hs], f == 0, f == 3) for f in range(4)],
                            atk + ["wdb%d" % s3], ["ps%d" % b])
                    cp("act" if half == 0 else "dve", ysb[x2][:, r, hs], psf(b), ["ps%d" % b], ["ysb%d.%d.%d" % (x2, r, half)])
            dma("pool", ys_d[e_ * CAPR:(e_ + 1) * CAPR, :].rearrange("(r p) d -> p r d", p=128), ysb[x2],
                ["ysb%d.%d.%d" % (x2, r, half) for r in range(2) for half in range(2)], ["ys.%d" % e_])

        for t_ in range(NE + 3):
            for off, stg in ((0, stageL), (1, stageT), (2, stageB), (3, stageC)):
                if 0 <= t_ - off < NE:
                    stg(t_ - off)
        yskeys = ["ys.%d" % e_ for e_ in range(NE)] + ["ys_trash"]
        checkpoint("p6b")
        P.barrier()
        A.release(*["wgb%d" % i for i in range(NWB)], *["wub%d" % i for i in range(NWB)], *["wdb%d" % i for i in range(NWB)],
                  "xbr0", "xbr1", "xbr2", "xbT0", "xbT1", "xbT2", "actT0", "actT1", "sgs0", "sgs1", "ysb0", "ysb1")

        x1r = [A.alloc("x1r%d" % i, [D], F32) for i in range(4)]
        g1b_ = [A.alloc("gy1_%d" % i, [D], F32) for i in range(4)]
        g2b_ = [A.alloc("gy2_%d" % i, [D], F32) for i in range(4)]
        for i in range(NT):
            x2 = i % 4
            dma("sp", x1r[x2], x1_d[i * 128:(i + 1) * 128, :], ["x1_d.%d" % i], ["x1r%d" % x2])
            for kk, gb_ in enumerate((g1b_[x2], g2b_[x2])):
                P.op("pool", lambda e, o_=gb_, ix=DST[:, i, kk:kk + 1]: e.indirect_dma_start(
                    out=o_, out_offset=None, in_=ys_d, in_offset=bass.IndirectOffsetOnAxis(ap=ix, axis=0)),
                    reads=yskeys + ["DST.a", "DST.b"], writes=["gy%d_%d" % (kk + 1, x2)], dma=True)
            stt(x1r[x2], g1b_[x2], WTS[:, i, 0:1], x1r[x2], ALU.mult, ALU.add, ["gy1_%d" % x2, "WTS.a", "x1r%d" % x2], ["x1r%d" % x2])
            stt(x1r[x2], g2b_[x2], WTS[:, i, 1:2], x1r[x2], ALU.mult, ALU.add, ["gy2_%d" % x2, "WTS.b", "x1r%d" % x2], ["x1r%d" % x2])
            dma("sp", out_d[i * 128:(i + 1) * 128, :], x1r[x2], ["x1r%d" % x2], ["out.%d" % i])
            outkeys.append("out.%d" % i)

        STAGE = 6
        P.frozen = False
        P.op("sp", lambda e: None, reads=outkeys)
        stats = P.emit(st)
    return nc, stats, list(dbg_out.keys())


_CACHE = {}


def prep_inputs(inputs):
    f = lambda a: np.ascontiguousarray(np.asarray(a, dtype=np.float32))
    shared = dict(
        cst=make_consts(),
        norm1_g=f(inputs["norm1_g"][0]), w_in=f(inputs["w_in"][0]),
        attn_q_gain=f(inputs["attn_q_gain"][0]), attn_k_gain=f(inputs["attn_k_gain"][0]),
        attn_sink=f(inputs["attn_sink"][0]), rel_bias_table=f(inputs["rel_bias_table"]),
        mlstm_conv_w=f(inputs["mlstm_conv_w"][0]), mlstm_conv_b=f(inputs["mlstm_conv_b"][0]),
        mlstm_gate_b=f(inputs["mlstm_gate_b"][0]).reshape(16), mlstm_out_gain=f(inputs["mlstm_out_gain"][0]).reshape(512),
        w_branch_attn=f(inputs["w_branch_attn"][0]), w_branch_mlstm=f(inputs["w_branch_mlstm"][0]),
        merge_b=f(inputs["merge_b"][0]), w_out=f(inputs["w_out"][0]), norm2_g=f(inputs["norm2_g"][0]),
        w_router_group=f(inputs["w_router_group"][0]), b_router_group=f(inputs["b_router_group"][0]),
        w_router_expert=f(inputs["w_router_expert"][0]), b_router_expert=f(inputs["b_router_expert"][0]),
        w_expert_gate=f(inputs["w_expert_gate"][0]), w_expert_up=f(inputs["w_expert_up"][0]),
        w_expert_down=f(inputs["w_expert_down"][0]),
    )
    return shared


def kernel(**inputs):
    x = np.asarray(inputs["x"], dtype=np.float32)
    nb = x.shape[0]
    if "nc" not in _CACHE:
        _CACHE["nc"] = build_program()[0]
    nc = _CACHE["nc"]
    shared = prep_inputs(inputs)
    in_maps = []
    for b in range(nb):
        m = dict(shared)
        m["x"] = np.ascontiguousarray(x[b])
        in_maps.append(m)
    res = run_bass_kernel_spmd(nc, in_maps, core_ids=list(range(nb)))
    return np.stack([np.asarray(r["out"], dtype=np.float32) for r in res.results], axis=0)
```
